# Optimizing a Trainium2 kernel written in Bass

```python
import jax, jax.numpy as jnp
from jax import lax
import numpy as np

D_MODEL = 1024
BATCH = 32
SEQ = 2048
DEPTH = 2

CHUNK = 64
CONV_CH = 512
CONV_K = 31
RET_HEADS = 4
RET_DK = 64
RET_DV = 128
RET_THETA = 10000.0
FOX_HEADS = 8
FOX_DH = 64
FOX_QBLOCK = 128
A_VAL = 0
A_GATE = A_VAL + CONV_CH
R_Q = A_GATE + CONV_CH
R_K = R_Q + RET_HEADS * RET_DK
R_V = R_K + RET_HEADS * RET_DK
R_G = R_V + RET_HEADS * RET_DV
F_Q = R_G + RET_HEADS * RET_DV
F_K = F_Q + FOX_HEADS * FOX_DH
F_V = F_K + FOX_HEADS * FOX_DH
F_F = F_V + FOX_HEADS * FOX_DH
IN_COLS = F_F + FOX_HEADS
N_BRANCH = 3
N_EXPERTS = 16
N_GROUPS = 4
EXPERTS_PER_GROUP = N_EXPERTS // N_GROUPS
TOP_K = 2
D_FF = 2048
MOE_BLOCK = 128
DN_ALPHA = (2 * DEPTH) ** 0.25
DN_BETA = (8 * DEPTH) ** -0.25
LN_EPS = 1e-5

kernel_name = "chunk_causal_hybrid_conv_retention_fox_grouped_moe"


def layer_norm(x, g=None, b=None):
    xf = x.astype(jnp.float32)
    mu = jnp.mean(xf, axis=-1, keepdims=True)
    var = jnp.mean(jnp.square(xf - mu), axis=-1, keepdims=True)
    y = (xf - mu) * lax.rsqrt(var + LN_EPS)
    if g is not None:
        y = y * g.astype(jnp.float32) + b.astype(jnp.float32)
    return y.astype(x.dtype)


def rotary(x):
    S, d = x.shape[1], x.shape[-1]
    half = d // 2
    inv = RET_THETA ** (-jnp.arange(half, dtype=jnp.float32) / half)
    ang = jnp.arange(S, dtype=jnp.float32)[:, None] * inv[None, :]
    cos = jnp.cos(ang)[None, :, None, :]
    sin = jnp.sin(ang)[None, :, None, :]
    x1, x2 = x[..., :half], x[..., half:]
    return jnp.concatenate([x1 * cos - x2 * sin, x1 * sin + x2 * cos], axis=-1)


def conv_module(a_val, a_gate, conv_w, conv_b, ln_g, ln_b):
    h = a_val * jax.nn.sigmoid(a_gate)
    h = lax.conv_general_dilated(
        h, conv_w[:, None, :].astype(h.dtype), window_strides=(1,),
        padding=[(CONV_K - 1, 0)], dimension_numbers=('NWC', 'WIO', 'NWC'),
        feature_group_count=CONV_CH) + conv_b.astype(h.dtype)
    return jax.nn.silu(layer_norm(h, ln_g, ln_b))


def retention(q, k, v):
    B, S, H, dk = q.shape
    dv = v.shape[-1]
    nc = S // CHUNK
    log_gamma = jnp.log1p(-jnp.exp2(-5.0 - jnp.arange(H, dtype=jnp.float32)))
    j = jnp.arange(CHUNK, dtype=jnp.float32)
    intra_decay = jnp.exp(log_gamma[:, None, None] * jnp.abs(j[:, None] - j[None, :]))
    q_decay = jnp.exp(log_gamma[None, :] * (j[:, None] + 1.0))[None, :, :, None]
    k_decay = jnp.exp(log_gamma[None, :] * (CHUNK - 1.0 - j[:, None]))[None, :, :, None]
    chunk_decay = jnp.exp(log_gamma * CHUNK)[None, :, None, None]
    qc = q.reshape(B, nc, CHUNK, H, dk)
    kc = k.reshape(B, nc, CHUNK, H, dk)
    vc = v.reshape(B, nc, CHUNK, H, dv)
    scores = jnp.einsum('bnjhd,bnlhd->bnhjl', qc, kc) * intra_decay
    intra = jnp.einsum('bnhjl,bnlhe->bnjhe', scores, vc)

    def step(state, inp):
        qn, kn, vn = inp
        cross = jnp.einsum('bjhd,bhde->bjhe', qn, state) * q_decay
        state = state * chunk_decay + jnp.einsum('bjhd,bjhe->bhde', kn * k_decay, vn)
        return state, cross

    state0 = jnp.zeros((B, H, dk, dv), q.dtype)
    _, cross = lax.scan(step, state0, (qc.transpose(1, 0, 2, 3, 4), kc.transpose(1, 0, 2, 3, 4),
                                        vc.transpose(1, 0, 2, 3, 4)))
    cross = cross.transpose(1, 0, 2, 3, 4)
    return (intra + cross).reshape(B, S, H, dv)


def forgetting_attention(q, k, v, f_logit):
    B, S, H, dh = q.shape
    log_f = jax.nn.log_sigmoid(f_logit.astype(jnp.float32))
    cum = jnp.cumsum(log_f, axis=1).transpose(0, 2, 1)
    scale = dh ** -0.5
    pos = jnp.arange(S)
    outs = []
    for i in range(S // FOX_QBLOCK):
        q0, q1 = i * FOX_QBLOCK, (i + 1) * FOX_QBLOCK
        logits = jnp.einsum('bqhd,bkhd->bhqk', q[:, q0:q1], k[:, :q1]).astype(jnp.float32) * scale
        logits = logits + cum[:, :, q0:q1, None] - cum[:, :, None, :q1]
        mask = pos[q0:q1, None] >= pos[None, :q1]
        logits = jnp.where(mask, logits, -jnp.inf)
        p = jax.nn.softmax(logits, axis=-1).astype(v.dtype)
        outs.append(jnp.einsum('bhqk,bkhd->bqhd', p, v[:, :q1]))
    return jnp.concatenate(outs, axis=1)


def route(xt, w_router, b_router):
    T = xt.shape[0]
    s = jax.nn.sigmoid(xt.astype(jnp.float32) @ w_router.astype(jnp.float32))
    sel = (s + b_router.astype(jnp.float32)).reshape(T, N_GROUPS, EXPERTS_PER_GROUP)
    group_score = jnp.sum(lax.top_k(sel, TOP_K)[0], axis=-1)
    g = jnp.argmax(group_score, axis=-1)
    sel_g = jnp.take_along_axis(sel, g[:, None, None], axis=1)[:, 0]
    _, local = lax.top_k(sel_g, TOP_K)
    idx = g[:, None] * EXPERTS_PER_GROUP + local
    w = jnp.take_along_axis(s, idx, axis=1)
    return idx, w / jnp.sum(w, axis=-1, keepdims=True)


def moe_ffn(u, w_router, b_router, w1, w3, w2):
    B, S, D = u.shape
    T = B * S
    xt = u.reshape(T, D)
    idx, wts = route(xt, w_router, b_router)
    A = T * TOP_K
    flat_e = idx.reshape(A)
    flat_w = wts.reshape(A)
    flat_tok = jnp.repeat(jnp.arange(T, dtype=jnp.int32), TOP_K)
    order = jnp.argsort(flat_e)
    se = flat_e[order]
    counts = jnp.bincount(flat_e, length=N_EXPERTS)
    start = jnp.cumsum(counts) - counts
    padded = (counts + MOE_BLOCK - 1) // MOE_BLOCK * MOE_BLOCK
    pend = jnp.cumsum(padded)
    pstart = pend - padded
    dest = pstart[se] + jnp.arange(A) - start[se]
    P = A + N_EXPERTS * MOE_BLOCK
    NB = P // MOE_BLOCK
    buf_tok = jnp.full((P,), T, jnp.int32).at[dest].set(flat_tok[order])
    buf_w = jnp.zeros((P,), jnp.float32).at[dest].set(flat_w[order])
    block_e = jnp.minimum(jnp.searchsorted(pend, jnp.arange(NB) * MOE_BLOCK, side='right'), N_EXPERTS - 1)
    x_pad = jnp.concatenate([xt, jnp.zeros((1, D), xt.dtype)], axis=0)
    xb = x_pad[buf_tok].reshape(NB, MOE_BLOCK, D)

    def expert_block(args):
        xblk, e = args
        h = jax.nn.silu(xblk @ w1[e]) * (xblk @ w3[e])
        return h @ w2[e]

    yb = lax.map(expert_block, (xb, block_e)).reshape(P, D)
    out = jnp.zeros((T + 1, D), yb.dtype).at[buf_tok].add(yb * buf_w[:, None].astype(yb.dtype))
    return out[:T].reshape(B, S, D)


def setup_inputs(seed: int = 0) -> dict:
    key = jax.random.key(seed)
    ks = jax.random.split(key, 32)
    f32 = jnp.float32
    L, D = DEPTH, D_MODEL

    def nrm(k, shape, fan_in, gain=1.0):
        return jax.random.normal(k, shape, f32) * (gain * fan_in ** -0.5)

    def small(k, shape, s=0.02):
        return jax.random.normal(k, shape, f32) * s

    return {
        "x": jax.random.normal(ks[0], (BATCH, SEQ, D), f32),
        "c": jax.random.normal(ks[1], (BATCH, D), f32),
        "w_ada": nrm(ks[2], (L, D, 6 * D), D, 0.5),
        "b_ada": small(ks[3], (L, 6 * D)),
        "w_in": nrm(ks[4], (L, D, IN_COLS), D),
        "conv_w": nrm(ks[5], (L, CONV_K, CONV_CH), CONV_K),
        "conv_b": small(ks[6], (L, CONV_CH)),
        "conv_ln_g": 1.0 + small(ks[7], (L, CONV_CH)),
        "conv_ln_b": small(ks[8], (L, CONV_CH)),
        "b_forget": jax.random.uniform(ks[9], (L, FOX_HEADS), f32, 1.0, 5.0),
        "w_conv_out": nrm(ks[10], (L, CONV_CH, D), CONV_CH),
        "w_ret_out": nrm(ks[11], (L, RET_HEADS * RET_DV, D), RET_HEADS * RET_DV),
        "w_fox_out": nrm(ks[12], (L, FOX_HEADS * FOX_DH, D), FOX_HEADS * FOX_DH),
        "w_gate": nrm(ks[13], (L, D, N_BRANCH * D), D),
        "b_gate": small(ks[14], (L, N_BRANCH * D)),
        "w_out": nrm(ks[15], (L, D, D), D, DN_BETA),
        "ln1_g": 1.0 + small(ks[16], (L, D)),
        "ln1_b": small(ks[17], (L, D)),
        "w_router": nrm(ks[18], (D, N_EXPERTS), D),
        "b_router": small(ks[19], (N_EXPERTS,), 0.01),
        "w1": nrm(ks[20], (L, N_EXPERTS, D, D_FF), D),
        "w3": nrm(ks[21], (L, N_EXPERTS, D, D_FF), D),
        "w2": nrm(ks[22], (L, N_EXPERTS, D_FF, D), D_FF, DN_BETA),
        "ln2_g": 1.0 + small(ks[23], (L, D)),
        "ln2_b": small(ks[24], (L, D)),
    }


def reference(x, c, w_ada, b_ada, w_in, conv_w, conv_b, conv_ln_g, conv_ln_b, b_forget,
              w_conv_out, w_ret_out, w_fox_out, w_gate, b_gate, w_out, ln1_g, ln1_b,
              w_router, b_router, w1, w3, w2, ln2_g, ln2_b):
    B, S, D = x.shape
    for l in range(DEPTH):
        ada = jax.nn.silu(c) @ w_ada[l] + b_ada[l]
        sh1, sc1, g1, sh2, sc2, g2 = jnp.split(ada[:, None, :], 6, axis=-1)

        u = layer_norm(x) * (1.0 + sc1) + sh1
        z = u @ w_in[l]

        y_a = conv_module(z[..., A_VAL:A_GATE], z[..., A_GATE:R_Q], conv_w[l], conv_b[l],
                          conv_ln_g[l], conv_ln_b[l]) @ w_conv_out[l]

        rq = rotary(z[..., R_Q:R_K].reshape(B, S, RET_HEADS, RET_DK).astype(jnp.float32))
        rk = rotary(z[..., R_K:R_V].reshape(B, S, RET_HEADS, RET_DK).astype(jnp.float32)) * (RET_DK ** -0.5)
        rv = z[..., R_V:R_G].reshape(B, S, RET_HEADS, RET_DV).astype(jnp.float32)
        ry = layer_norm(retention(rq, rk, rv)).reshape(B, S, RET_HEADS * RET_DV).astype(z.dtype)
        y_b = (jax.nn.silu(z[..., R_G:F_Q]) * ry) @ w_ret_out[l]

        fq = z[..., F_Q:F_K].reshape(B, S, FOX_HEADS, FOX_DH)
        fk = z[..., F_K:F_V].reshape(B, S, FOX_HEADS, FOX_DH)
        fv = z[..., F_V:F_F].reshape(B, S, FOX_HEADS, FOX_DH)
        f_logit = z[..., F_F:IN_COLS] + b_forget[l]
        y_c = forgetting_attention(fq, fk, fv, f_logit).reshape(B, S, FOX_HEADS * FOX_DH) @ w_fox_out[l]

        ga, gb, gc = jnp.split(jax.nn.sigmoid(u @ w_gate[l] + b_gate[l]), N_BRANCH, axis=-1)
        h = (ga * y_a + gb * y_b + gc * y_c) @ w_out[l]
        x = layer_norm(DN_ALPHA * x + g1 * h, ln1_g[l], ln1_b[l])

        u2 = layer_norm(x) * (1.0 + sc2) + sh2
        h2 = moe_ffn(u2, w_router, b_router, w1[l], w3[l], w2[l])
        x = layer_norm(DN_ALPHA * x + g2 * h2, ln2_g[l], ln2_b[l])
    return x
```

```python
import numpy as np
import ml_dtypes
from contextlib import ExitStack
import concourse.bass as bass
import concourse.mybir as mybir
from concourse.bass_utils import run_bass_kernel_spmd

F32, BF16 = mybir.dt.float32, mybir.dt.bfloat16
AF = mybir.ActivationFunctionType
ALU = mybir.AluOpType
AX = mybir.AxisListType

D = 1024
KC = 8
DEPTH = 2
CONV_CH, CONV_K = 512, 31
A_VAL, A_GATE, R_Q, R_K, R_V, R_G, F_Q, F_K, F_V, F_F, IN_COLS = 0, 512, 1024, 1280, 1536, 2048, 2560, 3072, 3584, 4096, 4104
NE, DFF = 16, 2048
DN_ALPHA = (2 * DEPTH) ** 0.25
EPS = 1e-5
NCORES = 8
NEG = -30000.0


class Dep:
    __slots__ = ("w", "r", "excl")

    def __init__(self, excl=False):
        self.w = None
        self.r = {}
        self.excl = excl


class Tr:
    def __init__(self, nc, es):
        self.nc, self.es = nc, es
        self.E = {"pe": nc.tensor, "act": nc.scalar, "dve": nc.vector, "pool": nc.gpsimd, "sp": nc.sync}
        self.sem, self.cnt = {}, {}
        for k in ("pe", "act", "dve", "pool"):
            self.newsem(k)
        self.known = {k: {} for k in self.E}
        self.nins = 0
        self.dead = False

    def newsem(self, key):
        self.sem[key] = self.es.enter_context(self.nc.semaphore("s_" + key))
        self.cnt[key] = 0

    def _deps(self, eng, r, w):
        need = {}
        for d in r:
            if d.w is not None:
                k, v = d.w
                if need.get(k, 0) < v:
                    need[k] = v
            if d.excl:
                for k, v in d.r.items():
                    if k != eng and need.get(k, 0) < v:
                        need[k] = v
        for d in w:
            if d.w is not None:
                k, v = d.w
                if need.get(k, 0) < v:
                    need[k] = v
            for k, v in d.r.items():
                if need.get(k, 0) < v:
                    need[k] = v
        kn = self.known[eng]
        for k, v in need.items():
            if k == "pe" and eng == "pe":
                continue
            if kn.get(k, 0) >= v:
                continue
            self.E[eng].wait_ge(self.sem[k], v)
            kn[k] = v

    def op(self, eng, fn, r=(), w=()):
        if self.dead:
            return
        self._deps(eng, r, w)
        ins = fn(self.E[eng])
        self.cnt[eng] += 1
        c = self.cnt[eng]
        ins.then_inc(self.sem[eng], 1)
        self.nins += 1
        for d in r:
            d.r[eng] = c
        for d in w:
            d.w = (eng, c)
            d.r = {}

    def dma(self, q, key, fn, r=(), w=()):
        if self.dead:
            return
        self._deps(q, r, w)
        ins = fn(self.E[q])
        self.cnt[key] += 16
        c = self.cnt[key]
        ins.then_inc(self.sem[key], 16)
        self.nins += 1
        for d in r:
            d.r[key] = c
        for d in w:
            d.w = (key, c)
            d.r = {}

    def barrier(self):
        if self.dead:
            return
        for eng in self.E:
            kn = self.known[eng]
            for k, c in self.cnt.items():
                if c > 0 and not (k == "pe" and eng == "pe") and kn.get(k, 0) < c:
                    self.E[eng].wait_ge(self.sem[k], c)
                    kn[k] = c


class Ring:
    def __init__(self, T, es, name, shapes, dtype, n, q, semname=None):
        self.T, self.n, self.q = T, n, q
        self.tiles = [[es.enter_context(T.nc.sbuf_tensor(f"{name}{i}_{j}", sh, dtype)) for j, sh in enumerate(shapes)]
                      for i in range(n)]
        self.deps = [Dep() for _ in range(n)]
        self.keys = [f"{semname or name}{i}" for i in range(n)]
        for k in self.keys:
            if k not in T.sem:
                T.newsem(k)
        self.pos = 0
        self.srcs = []
        self.base = 0
        self.issued = 0

    def begin(self, srcs):
        self.pos += len(self.srcs)
        self.base = self.pos
        self.srcs = srcs
        self.issued = 0

    def _issue(self, j):
        s = (self.base + j) % self.n
        pairs = self.srcs[j](self.tiles[s])
        for (o, i_) in pairs:
            self.T.dma(self.q, self.keys[s], (lambda e, o=o, i_=i_: e.dma_start(out=o, in_=i_)), w=[self.deps[s]])

    def issue_upto(self, j):
        j = min(j, len(self.srcs) - 1)
        while self.issued <= j:
            self._issue(self.issued)
            self.issued += 1

    def tile(self, i):
        assert i < self.issued
        s = (self.base + i) % self.n
        return self.tiles[s], self.deps[s]

    def get(self, i):
        lim = min(i + self.n - 1, len(self.srcs) - 1)
        while self.issued <= lim:
            self._issue(self.issued)
            self.issued += 1
        s = (self.base + i) % self.n
        return self.tiles[s], self.deps[s]


class _Stop(Exception):
    pass


def build(NSEQ, S, L, dump=None, stop=None):
    NT = S // 128
    NG = S // 512
    MU = min(S, 1024)
    NU = S // MU
    MT = MU // 128
    NSG = MU // 256
    nc = bass.Bass("TRN2", target_bir_lowering=False)

    def din(name, shape, dt=F32):
        return nc.dram_tensor(name, list(shape), dt, kind="ExternalInput").ap()

    x_in = din("x", [NSEQ, S, D])
    ct_in = din("ct", [128, KC, NSEQ])
    wada = din("wada", [L, 48, 128, KC, 128])
    bada = din("bada", [L, 128, 48])
    wp = din("wp", [L, 24, 128, KC, 128])
    wf = din("wf", [L, 128, KC, 8])
    wv = din("wv", [L, 128, KC, 1024])
    wm = din("wm", [L, 8, 128, 36, 128])
    bg = din("bg", [L, 128, 24])
    wo = din("wo", [L, 128, KC, 1024])
    cw = din("cw", [L, 128, 4, CONV_K])
    cprm = din("cprm", [L, 128, 3, 4])
    bfg = din("bfg", [L, 8, 1])
    lnp = din("lnp", [L, 4, 128, D])
    wr = din("wr", [128, KC, NE])
    brb = din("brb", [128, 8, NE])
    we1 = din("we1", [L, NE, 2, 128, KC, 1024])
    we3 = din("we3", [L, NE, 2, 128, KC, 1024])
    we2 = din("we2", [L, NE, 2, 128, KC, 1024])
    c_identb = din("c_identb", [128, 128], BF16)
    c_identf = din("c_identf", [128, 128])
    c_onesf = din("c_onesf", [128, 3, 128])
    c_perm = din("c_perm", [128, 128], BF16)
    c_cs = din("c_cs", [128, 2, S], BF16)
    c_dec = din("c_dec", [128, 20, 512], BF16)
    c_nmask = din("c_nmask", [128, 4, 512], BF16)
    c_sel8 = din("c_sel8", [8, 8, 128])
    c_sel16 = din("c_sel16", [16, 16, 128])
    c_onesb = din("c_onesb", [128, 64], BF16)
    c_gam = din("c_gam", [128, 64])
    out_d = nc.dram_tensor("out", [NSEQ, S, D], F32, kind="ExternalOutput").ap()
    xs = [nc.dram_tensor(f"xscr{i}", [NSEQ, S, D], F32, kind="Internal").ap() for i in range(2)]
    dumps = {}
    if dump:
        for nm, (shape, dt) in dump.items():
            dumps[nm] = nc.dram_tensor("dbg_" + nm, list(shape), dt, kind="ExternalOutput").ap()

    log_gamma = [float(np.log1p(-np.exp2(-5.0 - h))) for h in range(4)]

    with ExitStack() as es:
        T = Tr(nc, es)
        for k in ("const", "dbg", "decs", "wfs", "css", "lnps", "xld0", "xld1", "xst0", "xst1"):
            T.newsem(k)

        def sb(name, shape, dt=F32, stack=es):
            return stack.enter_context(nc.sbuf_tensor(name, list(shape), dt))

        PB = [es.enter_context(nc.psum_tensor(f"pb{i}", [128, 512], F32)) for i in range(7)]
        PBd = [Dep(True) for _ in range(7)]
        PT = es.enter_context(nc.psum_tensor("ptb", [128, 1024], BF16))
        PTd = Dep(True)

        identb = sb("identb", [128, 128], BF16)
        identf = sb("identf", [128, 128])
        onesf = sb("onesf", [128, 3, 128])
        permR = sb("permR", [128, 128], BF16)
        onesb = sb("onesb", [128, 64], BF16)
        neghalf = sb("neghalf", [128, 512])
        ones8 = sb("ones8", [8, 512])
        siluc = sb("siluc", [128, KC, NSEQ])
        wr_sb = sb("wr_sb", [128, KC, NE])
        brb_sb = sb("brb_sb", [128, 8, NE])
        cdep = Dep()
        for (o, i_) in ((identb, c_identb), (identf, c_identf), (onesf, c_onesf), (permR, c_perm), (onesb, c_onesb),
                        (siluc, ct_in), (wr_sb, wr), (brb_sb, brb)):
            T.dma("sp", "const", (lambda e, o=o, i_=i_: e.dma_start(out=o[:], in_=i_)), w=[cdep])
        T.op("dve", lambda e: e.memset(neghalf[:], -0.5), w=[cdep])
        T.op("dve", lambda e: e.memset(ones8[:], 1.0), w=[cdep])
        T.op("act", lambda e: e.activation(out=siluc[:], in_=siluc[:], func=AF.Silu), r=[cdep], w=[cdep])
        T.barrier()
        cdep = Dep()

        def dbg(name, ap, deps):
            if name in dumps:
                T.dma("sp", "dbg", (lambda e: e.dma_start(out=dumps[name], in_=ap)), r=deps)

        def rstd_from_var(var_ap, out_ap, tmp_dep, shape_cols):
            T.op("act", lambda e: e.activation(out=out_ap, in_=var_ap, func=AF.Ln, bias=EPS, scale=1.0), r=[tmp_dep], w=[tmp_dep])
            T.op("act", lambda e: e.activation(out=out_ap, in_=out_ap, func=AF.Exp, scale=-0.5), r=[tmp_dep], w=[tmp_dep])

        def ln_rows(xt, xdep, st_t, st_dep):
            T.op("dve", lambda e: e.bn_stats(out=st_t[:, 4:10], in_=xt[:, 0:512]), r=[xdep], w=[st_dep])
            T.op("dve", lambda e: e.bn_stats(out=st_t[:, 10:16], in_=xt[:, 512:1024]), r=[xdep], w=[st_dep])
            T.op("dve", lambda e: e.bn_aggr(out=st_t[:, 0:2], in_=st_t[:, 4:16]), r=[st_dep], w=[st_dep])
            rstd_from_var(st_t[:, 1:2], st_t[:, 2:3], st_dep, 1)

        def chk(tag):
            if stop == tag and not T.dead:
                T.barrier()
                T.dead = True

        xsrc = x_in
        try:
          for l in range(L):
              last_layer = (l == L - 1)
              x1 = xs[0]
              xdst = out_d if last_layer else xs[1]
              with ExitStack() as ls:
                  adaT = sb(f"adaT{l}", [128, 48, NSEQ], F32, ls)
                  bada_sb = sb(f"bada{l}", [128, 48], F32, ls)
                  cw_sb = sb(f"cw{l}", [128, 4, CONV_K], F32, ls)
                  cprm_sb = sb(f"cprm{l}", [128, 3, 4], F32, ls)
                  bfg_sb = sb(f"bfg{l}", [8, 1], F32, ls)
                  bg_sb = sb(f"bg{l}", [128, 24], F32, ls)
                  ldep = Dep()
                  T.dma("sp", "const", lambda e: e.dma_start(out=bada_sb[:], in_=bada[l]), w=[ldep])
                  T.dma("sp", "const", lambda e: e.dma_start(out=cw_sb[:], in_=cw[l]), w=[ldep])
                  T.dma("sp", "const", lambda e: e.dma_start(out=cprm_sb[:], in_=cprm[l]), w=[ldep])
                  T.dma("sp", "const", lambda e: e.dma_start(out=bfg_sb[:], in_=bfg[l]), w=[ldep])
                  T.dma("sp", "const", lambda e: e.dma_start(out=bg_sb[:], in_=bg[l]), w=[ldep])
                  T.op("dve", lambda e: e.tensor_scalar(out=bfg_sb[:], in0=bfg_sb[:], scalar1=-1.0, scalar2=None, op0=ALU.mult),
                       r=[ldep], w=[ldep])
                  with ExitStack() as ps_:
                      aring = Ring(T, ps_, f"ada{l}_", [[128, KC, 128]], F32, 3, "sp", "ada")
                      aring.begin([(lambda t, ch=ch: [(t[0][:], wada[l, ch])]) for ch in range(48)])
                      for ch in range(48):
                          wt, wd = aring.get(ch)
                          pb, pd = PB[ch % 2], PBd[ch % 2]
                          for kc in range(KC):
                              T.op("pe", (lambda e, kc=kc, wt=wt, pb=pb: e.matmul(pb[:, 0:NSEQ], lhsT=wt[0][:, kc, :], rhs=siluc[:, kc, :],
                                                                               start=(kc == 0), stop=(kc == KC - 1))),
                                   r=[wd], w=[pd])
                          one = 1.0 if (8 <= ch < 16 or 32 <= ch < 40) else 0.0
                          T.op("dve", (lambda e, ch=ch, pb=pb, one=one: e.tensor_scalar(out=adaT[:, ch, :], in0=pb[:, 0:NSEQ],
                                                                                     scalar1=bada_sb[:, ch:ch + 1], scalar2=one,
                                                                                     op0=ALU.add, op1=ALU.add)),
                               r=[pd, ldep], w=[ldep])
                      T.barrier()
                  T.barrier()

                  def build_gb(gb, gtmp, base, b, gbd, gtd):
                      for hh in range(2):
                          pb, pd = PB[hh], PBd[hh]
                          for c4 in range(4):
                              ch = hh * 4 + c4
                              T.op("dve", (lambda e, ch=ch: e.tensor_scalar(
                                  out=gtmp[:], in0=onesf[:, 0, :], scalar1=adaT[:, base + ch, b:b + 1], scalar2=None, op0=ALU.mult)),
                                  r=[ldep], w=[gtd])
                              T.op("pe", (lambda e, c4=c4, pb=pb: e.matmul(pb[:, c4 * 128:(c4 + 1) * 128], lhsT=gtmp[:], rhs=identf[:],
                                                                        start=True, stop=True)), r=[gtd], w=[pd])
                          T.op("act", (lambda e, hh=hh, pb=pb: e.activation(out=gb[:, hh * 512:(hh + 1) * 512], in_=pb[:], func=AF.Copy)),
                               r=[pd], w=[gbd])

                  chk("ada")
                  for b in range(NSEQ):
                      if True:
                          with ExitStack() as as_:
                              gb = sb(f"gba{l}_{b}", [128, D], F32, as_)
                              gtmp = sb(f"gtmpa{l}_{b}", [128, 128], F32, as_)
                              lnA = sb(f"lnA{l}_{b}", [128, 2, D], F32, as_)
                              dec = sb(f"dec{l}_{b}", [128, 20, 512], BF16, as_)
                              nmask = sb(f"nmask{l}_{b}", [128, 4, 512], BF16, as_)
                              cm8 = sb(f"cm8{l}_{b}", [8, 512], F32, as_)
                              rk = sb(f"rk{l}_{b}", [128, 2, S], BF16, as_)
                              fk = sb(f"fk{l}_{b}", [128, 4, S], BF16, as_)
                              rv = sb(f"rv{l}_{b}", [128, NT, 512], BF16, as_)
                              fv = sb(f"fv{l}_{b}", [128, NT, 512], BF16, as_)
                              ncumT = sb(f"ncumT{l}_{b}", [8, 512], F32, as_)
                              carry = sb(f"carry{l}_{b}", [8, 1], F32, as_)
                              ncumK = sb(f"ncumK{l}_{b}", [128, NT, 8], F32, as_)
                              hbuf = sb(f"hbuf{l}_{b}", [128, 4, 30 + 512], BF16, as_)
                              cs = sb(f"cs{l}_{b}", [128, 2, 512], BF16, as_)
                              uT = sb(f"uT{l}_{b}", [128, KC, 512], BF16, as_)
                              xg = [sb(f"xg{l}_{b}_{i}", [128, D], F32, as_) for i in range(2)]
                              xnb = sb(f"xnb{l}_{b}", [128, D], BF16, as_)
                              stt = sb(f"stt{l}_{b}", [128, 2, 16], F32, as_)
                              sig = sb(f"sig{l}_{b}", [128, 512], BF16, as_)
                              raw = sb(f"raw{l}_{b}", [128, 512], BF16, as_)
                              rt1 = sb(f"rt1{l}_{b}", [128, 512], F32, as_)
                              rt2 = sb(f"rt2{l}_{b}", [128, 512], F32, as_)
                              rq = sb(f"rq{l}_{b}", [128, 2, 512], BF16, as_)
                              fq = sb(f"fq{l}_{b}", [128, 4, 512], BF16, as_)
                              rgs = sb(f"rgs{l}_{b}", [128, 4, 512], BF16, as_)
                              yy = sb(f"yy{l}_{b}", [128, 12, 512], BF16, as_)
                              acc = sb(f"acc{l}_{b}", [128, 4, 512], F32, as_)
                              mrg = sb(f"mrg{l}_{b}", [128, KC, 512], BF16, as_)
                              mean_sb = sb(f"mean{l}_{b}", [128, 512], F32, as_)
                              rstd_sb = sb(f"rstd{l}_{b}", [128, 512], F32, as_)
                              pT = [sb(f"pT{l}_{b}_{i}", [128, 512], BF16, as_) for i in range(2)]
                              cq = sb(f"cq{l}_{b}", [128, 512], F32, as_)
                              lft = rt2[0:8, :]
                              sq = rt2
                              macc = rt1
                              sgt = cq
                              tmpf = [mean_sb, rstd_sb]
                              pring = Ring(T, as_, f"pr{l}_{b}_", [[128, KC, 128]], BF16, 3, "pool", "pr")
                              mring = Ring(T, as_, f"mr{l}_{b}_", [[128, 36, 128]], BF16, 1, "pool", "mr")
                              bigw = sb(f"bigw{l}_{b}", [128, KC, 512], BF16, as_)
                              wf_sb = sb(f"wf{l}_{b}", [128, KC, 8], BF16, as_)
                              bigk = "big"
                              if bigk not in T.sem:
                                  T.newsem(bigk)
                              (dec_d, hb_d, cs_d, uT_d, xnb_d, sig_d, raw_d, rt_d, rq_d, fq_d, rgs_d, acc_d, mean_d, rstd_d, cq_d, mrg_d,
                               big_d, wf_d, gbd, gtd, lnA_d, ncT_d, car_d, cm8_d) = [Dep() for _ in range(24)]
                              lft_d = rt_d
                              sq_d = rt_d
                              macc_d = rt_d
                              sgt_d = cq_d
                              tmpf_d = [mean_d, rstd_d]
                              xg_d = [Dep(), Dep()]
                              st_d = [Dep(), Dep()]
                              yy_d = [Dep() for _ in range(12)]
                              pT_d = [Dep(), Dep()]
                              rk_g = [Dep() for _ in range(NG)]
                              fk_g = [Dep() for _ in range(NG)]
                              rv_g = [Dep() for _ in range(NG)]
                              fv_g = [Dep() for _ in range(NG)]
                              ncK_g = [Dep() for _ in range(NG)]
                              T.dma("sp", "decs", lambda e: e.dma_start(out=dec[:], in_=c_dec), w=[dec_d])
                              T.dma("sp", "decs", lambda e: e.dma_start(out=nmask[:], in_=c_nmask), w=[dec_d])
                              T.dma("pool", "wfs", lambda e: e.dma_start(out=wf_sb[:], in_=wf[l]), w=[wf_d])
                              for j in range(2):
                                  T.dma("sp", "lnps", (lambda e, j=j: e.dma_start(out=lnA[:, j, :], in_=lnp[l, j])), w=[lnA_d])
                              T.op("dve", lambda e: e.memset(hbuf[:, :, 0:30], 0.0), w=[hb_d])
                              build_gb(gb, gtmp, 16, b, gbd, gtd)
                              pp_i = [0]

                              def pp():
                                  i = pp_i[0] % 2
                                  pp_i[0] += 1
                                  return PB[i], PBd[i]

                              sc_i = [0]

                              def scb():
                                  i = 2 + sc_i[0] % 2
                                  sc_i[0] += 1
                                  return PB[i], PBd[i]

                              xcnt = [0]

                              def xload(src_ap):
                                  i = xcnt[0] % 2
                                  xcnt[0] += 1
                                  T.dma("sp", f"xld{i}", (lambda e: e.dma_start(out=xg[i][:], in_=src_ap)), w=[xg_d[i]])
                                  return i

                              for g in range(NG):
                                  t0 = g * 512
                                  T.dma("sp", "css", (lambda e, t0=t0: e.dma_start(out=cs[:], in_=c_cs[:, :, t0:t0 + 512])), w=[cs_d])
                                  for tt in range(4):
                                      xi = xload(xsrc[b, t0 + tt * 128:t0 + (tt + 1) * 128, :])
                                      ln_rows(xg[xi][:], xg_d[xi], stt[:, xi, :], st_d[xi])
                                      T.op("dve", (lambda e, xi=xi: e.tensor_scalar(out=xnb[:], in0=xg[xi][:], scalar1=stt[:, xi, 0:1],
                                                                                 scalar2=stt[:, xi, 2:3], op0=ALU.subtract, op1=ALU.mult)),
                                           r=[xg_d[xi], st_d[xi]], w=[xnb_d])
                                      for kc in range(KC):
                                          T.op("pe", (lambda e, kc=kc: e.transpose(out=PT[:, kc * 128:(kc + 1) * 128], in_=xnb[:, kc * 128:(kc + 1) * 128],
                                                                                   identity=identb[:])), r=[xnb_d], w=[PTd])
                                      for kc in range(KC):
                                          T.op("act", (lambda e, kc=kc, tt=tt: e.activation(out=uT[:, kc, tt * 128:(tt + 1) * 128],
                                                                                          in_=PT[:, kc * 128:(kc + 1) * 128], func=AF.Identity,
                                                                                          bias=adaT[:, kc, b:b + 1], scale=adaT[:, 8 + kc, b:b + 1])),
                                               r=[PTd, ldep], w=[uT_d])
                                  if g == 0 and b == 0:
                                      dbg(f"uT{l}", uT[:], [uT_d])

                                  chk("A")
                                  pring.begin([(lambda t, ci=ci: [(t[0][:], wp[l, ci])]) for ci in range(24)])

                                  def proj(ci):
                                      wt, wd = pring.get(ci)
                                      pb, pd = pp()
                                      for kc in range(KC):
                                          T.op("pe", (lambda e, kc=kc, wt=wt, pb=pb: e.matmul(pb[:], lhsT=wt[0][:, kc, :], rhs=uT[:, kc, :],
                                                                                           start=(kc == 0), stop=(kc == KC - 1))),
                                               r=[wd, uT_d], w=[pd])
                                      return pb, pd

                                  def rotary(pb, pd, dst_ap, dst_deps):
                                      T.op("act", lambda e: e.activation(out=raw[:], in_=pb[:], func=AF.Copy), r=[pd], w=[raw_d])
                                      p2, p2d = pp()
                                      T.op("pe", lambda e: e.matmul(p2[:], lhsT=permR[:], rhs=raw[:], start=True, stop=True), r=[raw_d], w=[p2d])
                                      T.op("act", lambda e: e.activation(out=rt1[:], in_=pb[:], func=AF.Copy), r=[pd], w=[rt_d])
                                      T.op("act", lambda e: e.activation(out=rt2[:], in_=p2[:], func=AF.Copy), r=[p2d], w=[rt_d])
                                      T.op("dve", lambda e: e.tensor_tensor(out=rt1[:], in0=rt1[:], in1=cs[:, 0, :], op=ALU.mult), r=[rt_d, cs_d], w=[rt_d])
                                      T.op("dve", lambda e: e.tensor_tensor(out=rt2[:], in0=rt2[:], in1=cs[:, 1, :], op=ALU.mult), r=[rt_d, cs_d], w=[rt_d])
                                      T.op("dve", lambda e: e.tensor_tensor(out=dst_ap, in0=rt1[:], in1=rt2[:], op=ALU.add), r=[rt_d], w=dst_deps)

                                  for c in range(4):
                                      pb, pd = proj(2 * c)
                                      T.op("act", (lambda e, pb=pb: e.activation(out=sig[:], in_=pb[:], func=AF.Sigmoid)), r=[pd], w=[sig_d])
                                      pb, pd = proj(2 * c + 1)
                                      T.op("dve", (lambda e, pb=pb, c=c: e.tensor_tensor(out=hbuf[:, c, 30:542], in0=pb[:], in1=sig[:], op=ALU.mult)),
                                           r=[pd, sig_d], w=[hb_d])
                                  chk("B1")
                                  for c in range(2):
                                      pb, pd = proj(8 + c)
                                      rotary(pb, pd, rk[:, c, t0:t0 + 512], [rk_g[g]])
                                  chk("B2")
                                  for c in range(4):
                                      pb, pd = proj(10 + c)
                                      T.op("act", (lambda e, pb=pb, c=c: e.activation(out=fk[:, c, t0:t0 + 512], in_=pb[:], func=AF.Copy)), r=[pd], w=[fk_g[g]])
                                  for c in range(2):
                                      pb, pd = proj(14 + c)
                                      rotary(pb, pd, rq[:, c, :], [rq_d])
                                  for c in range(4):
                                      pb, pd = proj(16 + c)
                                      T.op("act", (lambda e, pb=pb, c=c: e.activation(out=fq[:, c, :], in_=pb[:], func=AF.Copy)), r=[pd], w=[fq_d])
                                  for c in range(4):
                                      pb, pd = proj(20 + c)
                                      T.op("act", (lambda e, pb=pb, c=c: e.activation(out=rgs[:, c, :], in_=pb[:], func=AF.Silu)), r=[pd], w=[rgs_d])
                                  chk("B3")
                                  pb, pd = pp()
                                  for kc in range(KC):
                                      T.op("pe", (lambda e, kc=kc, pb=pb: e.matmul(pb[0:8, :], lhsT=wf_sb[:, kc, :], rhs=uT[:, kc, :],
                                                                                start=(kc == 0), stop=(kc == KC - 1))), r=[wf_d, uT_d], w=[pd])
                                  T.op("act", (lambda e, pb=pb: e.activation(out=lft, in_=pb[0:8, :], func=AF.Exp, bias=bfg_sb[:, 0:1], scale=-1.0)),
                                       r=[pd, ldep], w=[lft_d])
                                  T.op("act", lambda e: e.activation(out=lft, in_=lft, func=AF.Ln, bias=1.0, scale=1.0), r=[lft_d], w=[lft_d])
                                  if g > 0:
                                      T.op("act", lambda e: e.activation(out=carry[:], in_=ncumT[:, 511:512], func=AF.Copy), r=[ncT_d], w=[car_d])
                                  init = 0.0 if g == 0 else carry[:, 0:1]
                                  T.op("dve", (lambda e, init=init: e.tensor_tensor_scan(out=ncumT[:], data0=ones8[:], data1=lft,
                                                                                      initial=init, op0=ALU.mult, op1=ALU.add)),
                                       r=[lft_d, car_d], w=[ncT_d])
                                  pb, pd = pp()
                                  for tt in range(4):
                                      T.op("pe", (lambda e, tt=tt, pb=pb: e.transpose(out=pb[:, tt * 8:(tt + 1) * 8],
                                                                                   in_=ncumT[:, tt * 128:(tt + 1) * 128],
                                                                                   identity=identf[0:8, 0:8])), r=[ncT_d], w=[pd])
                                  T.op("act", (lambda e, pb=pb, g=g: e.activation(out=ncumK[:, 4 * g:4 * g + 4, :],
                                                                               in_=pb[:, 0:32].rearrange("p (a b) -> p a b", b=8), func=AF.Copy)),
                                       r=[pd], w=[ncK_g[g]])
                                  chk("B4")
                                  for hv in range(2):
                                      T.dma("pool", bigk, (lambda e, hv=hv: e.dma_start(out=bigw[:], in_=wv[l, :, :, hv * 512:(hv + 1) * 512])), w=[big_d])
                                      for tt in range(4):
                                          pb, pd = pp()
                                          for kc in range(KC):
                                              T.op("pe", (lambda e, kc=kc, pb=pb, tt=tt: e.matmul(
                                                  pb[:], lhsT=uT[:, kc, tt * 128:(tt + 1) * 128], rhs=bigw[:, kc, :],
                                                  start=(kc == 0), stop=(kc == KC - 1))), r=[big_d, uT_d], w=[pd])
                                          dst = rv if hv == 0 else fv
                                          dd = rv_g[g] if hv == 0 else fv_g[g]
                                          T.op("act", (lambda e, pb=pb, dst=dst, tt=tt, g=g: e.activation(out=dst[:, 4 * g + tt, :], in_=pb[:], func=AF.Copy)),
                                               r=[pd], w=[dd])

                                  chk("B")
                                  for c in range(4):
                                      T.op("dve", (lambda e, c=c: e.tensor_scalar(out=acc[:, c, :], in0=hbuf[:, c, 0:512], scalar1=cw_sb[:, c, 0:1],
                                                                               scalar2=cprm_sb[:, 0, c:c + 1], op0=ALU.mult, op1=ALU.add)),
                                           r=[hb_d, ldep], w=[acc_d])
                                  for k in range(1, CONV_K):
                                      for c in range(4):
                                          T.op("dve", (lambda e, c=c, k=k: e.scalar_tensor_tensor(out=acc[:, c, :], in0=hbuf[:, c, k:k + 512],
                                                                                                scalar=cw_sb[:, c, k:k + 1], in1=acc[:, c, :],
                                                                                                op0=ALU.mult, op1=ALU.add)),
                                               r=[hb_d, acc_d], w=[acc_d])
                                  for c in range(4):
                                      T.op("act", (lambda e, c=c: e.activation(out=hbuf[:, c, 0:30], in_=hbuf[:, c, 512:542], func=AF.Copy)),
                                           r=[hb_d], w=[hb_d])

                                  def ln_feat(chunks, cdeps, ones_ap, nch):
                                      pm, pmd = pp()
                                      for i, ch in enumerate(chunks):
                                          T.op("pe", (lambda e, i=i, ch=ch: e.matmul(pm[:], lhsT=ones_ap, rhs=ch, start=(i == 0), stop=(i == nch - 1))),
                                               r=cdeps, w=[pmd])
                                      pe2, pe2d = pp()
                                      for i, ch in enumerate(chunks):
                                          T.op("act", (lambda e, ch=ch: e.activation(out=sq[:], in_=ch, func=AF.Square)), r=cdeps, w=[sq_d])
                                          T.op("pe", (lambda e, i=i: e.matmul(pe2[:], lhsT=ones_ap, rhs=sq[:], start=(i == 0), stop=(i == nch - 1))),
                                               r=[sq_d], w=[pe2d])
                                      T.op("act", lambda e: e.activation(out=mean_sb[:], in_=pm[:], func=AF.Copy), r=[pmd], w=[mean_d])
                                      T.op("dve", lambda e: e.tensor_tensor(out=rstd_sb[:], in0=mean_sb[:], in1=mean_sb[:], op=ALU.mult), r=[mean_d], w=[rstd_d])
                                      T.op("dve", lambda e: e.tensor_tensor(out=rstd_sb[:], in0=pe2[:], in1=rstd_sb[:], op=ALU.subtract), r=[pe2d, rstd_d], w=[rstd_d])
                                      rstd_from_var(rstd_sb[:], rstd_sb[:], rstd_d, 512)

                                  ln_feat([acc[:, c, :] for c in range(4)], [acc_d], onesf[:, 1, :], 4)
                                  for c in range(4):
                                      T.op("dve", (lambda e, c=c: e.tensor_tensor(out=acc[:, c, :], in0=acc[:, c, :], in1=mean_sb[:], op=ALU.subtract)),
                                           r=[acc_d, mean_d], w=[acc_d])
                                      T.op("dve", (lambda e, c=c: e.tensor_tensor(out=acc[:, c, :], in0=acc[:, c, :], in1=rstd_sb[:], op=ALU.mult)),
                                           r=[acc_d, rstd_d], w=[acc_d])
                                      T.op("act", (lambda e, c=c: e.activation(out=yy[:, c, :], in_=acc[:, c, :], func=AF.Silu,
                                                                            bias=cprm_sb[:, 2, c:c + 1], scale=cprm_sb[:, 1, c:c + 1])),
                                           r=[acc_d, ldep], w=[yy_d[c]])

                                  chk("C")
                                  nkt = 4 * g + 4
                                  for h in range(4):
                                      c, r0 = h // 2, (h % 2) * 64
                                      po, pod = PB[4], PBd[4]
                                      def retS(j):
                                          ps, psd = scb()
                                          T.op("pe", (lambda e: e.matmul(ps[:], lhsT=rk[r0:r0 + 64, c, j * 128:(j + 1) * 128],
                                                                        rhs=rq[r0:r0 + 64, c, :], start=True, stop=True)),
                                               r=[rk_g[j // 4], rq_d], w=[psd])
                                          pi = j % 2
                                          if j < 4 * g:
                                              sc = 0.125 * float(np.exp(log_gamma[h] * 128.0 * (4 * g - j)))
                                              dt_ = dec[:, h, :]
                                          else:
                                              sc = 0.125
                                              dt_ = dec[:, 4 + 4 * h + (j - 4 * g), :]
                                          T.op("dve", (lambda e: e.scalar_tensor_tensor(out=pT[pi][:], in0=ps[:], scalar=sc, in1=dt_,
                                                                                       op0=ALU.mult, op1=ALU.mult)),
                                               r=[psd, dec_d], w=[pT_d[pi]])

                                      def retV(j):
                                          pi = j % 2
                                          T.op("pe", (lambda e: e.matmul(po[:], lhsT=rv[:, j, h * 128:(h + 1) * 128], rhs=pT[pi][:],
                                                                        start=(j == 0), stop=(j == nkt - 1))),
                                               r=[rv_g[j // 4], pT_d[pi]], w=[pod])

                                      for j in range(nkt):
                                          retS(j)
                                          if j > 0:
                                              retV(j - 1)
                                      retV(nkt - 1)
                                      T.op("act", lambda e: e.activation(out=acc[:, 0, :], in_=po[:], func=AF.Copy), r=[pod], w=[acc_d])
                                      ln_feat([acc[:, 0, :]], [acc_d], onesf[:, 2, :], 1)
                                      T.op("dve", lambda e: e.tensor_tensor(out=acc[:, 0, :], in0=acc[:, 0, :], in1=mean_sb[:], op=ALU.subtract),
                                           r=[acc_d, mean_d], w=[acc_d])
                                      T.op("dve", lambda e: e.tensor_tensor(out=acc[:, 0, :], in0=acc[:, 0, :], in1=rstd_sb[:], op=ALU.mult),
                                           r=[acc_d, rstd_d], w=[acc_d])
                                      T.op("dve", (lambda e, h=h: e.tensor_tensor(out=yy[:, 4 + h, :], in0=acc[:, 0, :], in1=rgs[:, h, :], op=ALU.mult)),
                                           r=[acc_d, rgs_d], w=[yy_d[4 + h]])

                                  chk("D")
                                  for h in range(8):
                                      c, r0 = h // 2, (h % 2) * 64
                                      pn, pnd, pdn, pdnd = PB[4], PBd[4], PB[5], PBd[5]
                                      pb, pd = pp()
                                      T.op("dve", (lambda e, h=h: e.tensor_scalar(out=cm8[:], in0=ncumT[:], scalar1=identf[0:8, h:h + 1], scalar2=None, op0=ALU.mult)),
                                           r=[ncT_d], w=[cm8_d])
                                      T.op("pe", (lambda e, pb=pb: e.matmul(pb[:], lhsT=onesf[0:8, 0, :], rhs=cm8[:], start=True, stop=True)),
                                           r=[cm8_d], w=[pd])
                                      T.op("act", (lambda e, pb=pb: e.activation(out=cq[:], in_=pb[:], func=AF.Copy, scale=-1.0)), r=[pd], w=[cq_d])
                                      def foxS(j):
                                          ps, psd = scb()
                                          T.op("pe", (lambda e: e.matmul(ps[:], lhsT=fk[r0:r0 + 64, c, j * 128:(j + 1) * 128],
                                                                        rhs=fq[r0:r0 + 64, c, :], start=True, stop=True)),
                                               r=[fk_g[j // 4], fq_d], w=[psd])
                                          pi = j % 2
                                          T.op("dve", (lambda e: e.scalar_tensor_tensor(out=tmpf[pi][:], in0=ps[:], scalar=0.125, in1=cq[:],
                                                                                       op0=ALU.mult, op1=ALU.add)),
                                               r=[psd, cq_d], w=[tmpf_d[pi]])
                                          if j >= 4 * g:
                                              T.op("dve", (lambda e: e.tensor_tensor(out=tmpf[pi][:], in0=tmpf[pi][:], in1=nmask[:, j - 4 * g, :], op=ALU.add)),
                                                   r=[tmpf_d[pi], dec_d], w=[tmpf_d[pi]])
                                          T.op("act", (lambda e: e.activation(out=pT[pi][:], in_=tmpf[pi][:], func=AF.Exp,
                                                                             bias=ncumK[:, j, h:h + 1], scale=1.0)),
                                               r=[tmpf_d[pi], ncK_g[j // 4]], w=[pT_d[pi]])

                                      def foxV(j):
                                          pi = j % 2
                                          T.op("pe", (lambda e: e.matmul(pn[r0:r0 + 64, :], lhsT=fv[:, j, h * 64:(h + 1) * 64], rhs=pT[pi][:],
                                                                        start=(j == 0), stop=(j == nkt - 1))),
                                               r=[fv_g[j // 4], pT_d[pi]], w=[pnd])
                                          T.op("pe", (lambda e: e.matmul(pdn[r0:r0 + 64, :], lhsT=onesb[:, 0:64], rhs=pT[pi][:],
                                                                        start=(j == 0), stop=(j == nkt - 1))),
                                               r=[pT_d[pi]], w=[pdnd])

                                      for j in range(nkt):
                                          foxS(j)
                                          if j > 0:
                                              foxV(j - 1)
                                      foxV(nkt - 1)
                                      if h % 2 == 1:
                                          T.op("dve", lambda e: e.reciprocal(out=rt1[:], in_=pdn[:]), r=[pdnd], w=[rt_d])
                                          T.op("dve", (lambda e, c=c: e.tensor_tensor(out=yy[:, 8 + c, :], in0=pn[:], in1=rt1[:], op=ALU.mult)),
                                               r=[pnd, rt_d], w=[yy_d[8 + c]])
                                  if g == 0 and b == 0:
                                      dbg(f"yy{l}", yy[:], yy_d)

                                  chk("E")
                                  mring.begin([(lambda t, dc=dc: [(t[0][:], wm[l, dc])]) for dc in range(8)])
                                  for dc in range(8):
                                      wt, wd = mring.get(dc)
                                      for br in range(3):
                                          pa, pad = pp()
                                          for kc in range(4):
                                              T.op("pe", (lambda e, kc=kc, br=br, pa=pa, wt=wt: e.matmul(pa[:], lhsT=wt[0][:, br * 4 + kc, :], rhs=yy[:, br * 4 + kc, :],
                                                                                                    start=(kc == 0), stop=(kc == 3))),
                                                   r=[wd, yy_d[br * 4 + kc]], w=[pad])
                                          pg, pgd = pp()
                                          for kc in range(KC):
                                              T.op("pe", (lambda e, kc=kc, br=br, pg=pg, wt=wt: e.matmul(pg[:], lhsT=wt[0][:, 12 + br * 8 + kc, :], rhs=uT[:, kc, :],
                                                                                                    start=(kc == 0), stop=(kc == KC - 1))),
                                                   r=[wd, uT_d], w=[pgd])
                                          T.op("act", (lambda e, pg=pg, br=br, dc=dc: e.activation(out=sgt[:], in_=pg[:], func=AF.Sigmoid,
                                                                                                bias=bg_sb[:, br * 8 + dc:br * 8 + dc + 1], scale=1.0)),
                                               r=[pgd, ldep], w=[sgt_d])
                                          if br == 0:
                                              T.op("dve", (lambda e, pa=pa: e.tensor_tensor(out=macc[:], in0=pa[:], in1=sgt[:], op=ALU.mult)),
                                                   r=[pad, sgt_d], w=[macc_d])
                                          else:
                                              T.op("dve", (lambda e, pa=pa: e.tensor_tensor(out=sgt[:], in0=pa[:], in1=sgt[:], op=ALU.mult)),
                                                   r=[pad, sgt_d], w=[sgt_d])
                                              if br == 1:
                                                  T.op("dve", lambda e: e.tensor_tensor(out=macc[:], in0=macc[:], in1=sgt[:], op=ALU.add),
                                                       r=[macc_d, sgt_d], w=[macc_d])
                                              else:
                                                  T.op("dve", (lambda e, dc=dc: e.tensor_tensor(out=mrg[:, dc, :], in0=macc[:], in1=sgt[:], op=ALU.add)),
                                                       r=[macc_d, sgt_d], w=[mrg_d])

                                  chk("F")
                                  T.dma("pool", bigk, lambda e: e.dma_start(out=bigw[:], in_=wo[l, :, :, 0:512]), w=[big_d])
                                  for tt in range(4):
                                      pb, pd = pp()
                                      for kc in range(KC):
                                          T.op("pe", (lambda e, kc=kc, pb=pb, tt=tt: e.matmul(
                                              pb[:], lhsT=mrg[:, kc, tt * 128:(tt + 1) * 128], rhs=bigw[:, kc, :],
                                              start=(kc == 0), stop=(kc == KC - 1))), r=[big_d, mrg_d], w=[pd])
                                      T.op("dve", (lambda e, pb=pb, tt=tt: e.tensor_tensor(out=acc[:, tt, :], in0=pb[:], in1=gb[:, 0:512], op=ALU.mult)),
                                           r=[pd, gbd], w=[acc_d])
                                  T.dma("pool", bigk, lambda e: e.dma_start(out=bigw[:], in_=wo[l, :, :, 512:1024]), w=[big_d])
                                  for tt in range(4):
                                      pb, pd = pp()
                                      for kc in range(KC):
                                          T.op("pe", (lambda e, kc=kc, pb=pb, tt=tt: e.matmul(
                                              pb[:], lhsT=mrg[:, kc, tt * 128:(tt + 1) * 128], rhs=bigw[:, kc, :],
                                              start=(kc == 0), stop=(kc == KC - 1))), r=[big_d, mrg_d], w=[pd])
                                      T.op("dve", (lambda e, pb=pb: e.tensor_tensor(out=rt1[:], in0=pb[:], in1=gb[:, 512:1024], op=ALU.mult)),
                                           r=[pd, gbd], w=[rt_d])
                                      xi = xload(xsrc[b, t0 + tt * 128:t0 + (tt + 1) * 128, :])
                                      T.op("dve", (lambda e, tt=tt, xi=xi: e.scalar_tensor_tensor(out=xg[xi][:, 0:512], in0=xg[xi][:, 0:512], scalar=DN_ALPHA,
                                                                                                in1=acc[:, tt, :], op0=ALU.mult, op1=ALU.add)),
                                           r=[acc_d, xg_d[xi]], w=[xg_d[xi]])
                                      T.op("dve", (lambda e, xi=xi: e.scalar_tensor_tensor(out=xg[xi][:, 512:1024], in0=xg[xi][:, 512:1024], scalar=DN_ALPHA,
                                                                                         in1=rt1[:], op0=ALU.mult, op1=ALU.add)),
                                           r=[rt_d, xg_d[xi]], w=[xg_d[xi]])
                                      ln_rows(xg[xi][:], xg_d[xi], stt[:, xi, :], st_d[xi])
                                      T.op("dve", (lambda e, xi=xi: e.tensor_scalar(out=xg[xi][:], in0=xg[xi][:], scalar1=stt[:, xi, 0:1],
                                                                                 scalar2=stt[:, xi, 2:3], op0=ALU.subtract, op1=ALU.mult)),
                                           r=[xg_d[xi], st_d[xi]], w=[xg_d[xi]])
                                      T.op("dve", (lambda e, xi=xi: e.tensor_tensor(out=xg[xi][:], in0=xg[xi][:], in1=lnA[:, 0, :], op=ALU.mult)),
                                           r=[xg_d[xi], lnA_d], w=[xg_d[xi]])
                                      T.op("dve", (lambda e, xi=xi: e.tensor_tensor(out=xg[xi][:], in0=xg[xi][:], in1=lnA[:, 1, :], op=ALU.add)),
                                           r=[xg_d[xi], lnA_d], w=[xg_d[xi]])
                                      if g == 0 and b == 0 and tt == 0:
                                          dbg(f"x1_{l}", xg[xi][:], [xg_d[xi]])
                                      T.dma("sp", f"xst{xi}", (lambda e, tt=tt, t0=t0, xi=xi: e.dma_start(out=x1[b, t0 + tt * 128:t0 + (tt + 1) * 128, :], in_=xg[xi][:])),
                                            r=[xg_d[xi]])
                              T.barrier()
                          T.barrier()

                          chk("G")
                          with ExitStack() as ms:
                              gb = sb(f"gbm{l}_{b}", [128, D], F32, ms)
                              gtmp = sb(f"gtmpm{l}_{b}", [128, 128], F32, ms)
                              lnM = sb(f"lnM{l}_{b}", [128, 2, D], F32, ms)
                              u2T = sb(f"u2T{l}_{b}", [128, KC, MU], BF16, ms)
                              u2f = sb(f"u2f{l}_{b}", [128, KC, 128], F32, ms)
                              xt = [sb(f"xt{l}_{b}_{i}", [128, D], F32, ms) for i in range(2)]
                              xn = sb(f"xn{l}_{b}", [128, D], F32, ms)
                              stm = sb(f"stm{l}_{b}", [128, 2, 16], F32, ms)
                              yacc = sb(f"yacc{l}_{b}", [128, MT, D], F32, ms)
                              rS = sb(f"rS{l}_{b}", [128, MT, NE], F32, ms)
                              rSel = sb(f"rSel{l}_{b}", [128, MT, NE], F32, ms)
                              rP6 = sb(f"rP6{l}_{b}", [128, MT * 4, 6], F32, ms)
                              rG = sb(f"rG{l}_{b}", [128, MT, 4], F32, ms)
                              rGm = sb(f"rGm{l}_{b}", [128, MT], F32, ms)
                              rPen = sb(f"rPen{l}_{b}", [128, MT, 4], F32, ms)
                              rT8 = sb(f"rT8{l}_{b}", [128, MT, 8], F32, ms)
                              rW = sb(f"rW{l}_{b}", [128, MT, NE], F32, ms)
                              rSum = sb(f"rSum{l}_{b}", [128, MT], F32, ms)
                              wT = sb(f"wT{l}_{b}", [16, MU], F32, ms)
                              wTe = sb(f"wTe{l}_{b}", [16, 512], F32, ms)
                              wb_sb = [sb(f"wb{l}_{b}_{i}", [128, MU], BF16, ms) for i in range(2)]
                              silt = [sb(f"silt{l}_{b}_{i}", [128, 256], F32, ms) for i in range(2)]
                              hT = [sb(f"hT{l}_{b}_{i}", [128, 256], BF16, ms) for i in range(2)]
                              ering = Ring(T, ms, f"ex{l}_{b}_", [[128, KC, 1024]] * 3, BF16, 2, "pool", "ex")
                              (u2T_d, u2f_d, xn_d, yacc_d, rt_d, wT_d, wTe_d, gbd, gtd, lnM_d) = [Dep() for _ in range(10)]
                              xt_d = [Dep(), Dep()]
                              stm_d = [Dep(), Dep()]
                              wb_d = [Dep(), Dep()]
                              silt_d = [Dep(), Dep()]
                              hT_d = [Dep(), Dep()]
                              for j in range(2):
                                  T.dma("sp", "lnps", (lambda e, j=j: e.dma_start(out=lnM[:, j, :], in_=lnp[l, 2 + j])), w=[lnM_d])
                              build_gb(gb, gtmp, 40, b, gbd, gtd)
                              for u in range(NU):
                                  u0 = u * MU
                                  for ti in range(MT):
                                      xi = ti % 2
                                      T.dma("sp", f"xld{xi}", (lambda e, xi=xi, ti=ti: e.dma_start(out=xt[xi][:], in_=x1[b, u0 + ti * 128:u0 + (ti + 1) * 128, :])),
                                            w=[xt_d[xi]])
                                      ln_rows(xt[xi][:], xt_d[xi], stm[:, xi, :], stm_d[xi])
                                      T.op("dve", (lambda e, xi=xi: e.tensor_scalar(out=xn[:], in0=xt[xi][:], scalar1=stm[:, xi, 0:1], scalar2=stm[:, xi, 2:3],
                                                                                 op0=ALU.subtract, op1=ALU.mult)), r=[xt_d[xi], stm_d[xi]], w=[xn_d])
                                      for kc in range(KC):
                                          pbk, pbd = PB[4 + kc // 4], PBd[4 + kc // 4]
                                          T.op("pe", (lambda e, kc=kc, pbk=pbk: e.transpose(out=pbk[:, (kc % 4) * 128:(kc % 4 + 1) * 128], in_=xn[:, kc * 128:(kc + 1) * 128],
                                                                                         identity=identf[:])), r=[xn_d], w=[pbd])
                                      for kc in range(KC):
                                          pbk, pbd = PB[4 + kc // 4], PBd[4 + kc // 4]
                                          T.op("act", (lambda e, kc=kc, pbk=pbk: e.activation(out=u2f[:, kc, :], in_=pbk[:, (kc % 4) * 128:(kc % 4 + 1) * 128], func=AF.Identity,
                                                                                           bias=adaT[:, 24 + kc, b:b + 1], scale=adaT[:, 32 + kc, b:b + 1])),
                                               r=[pbd, ldep], w=[u2f_d])
                                      T.op("dve", (lambda e, ti=ti: e.tensor_copy(out=u2T[:, :, ti * 128:(ti + 1) * 128], in_=u2f[:])), r=[u2f_d], w=[u2T_d])
                                      for kc in range(KC):
                                          T.op("pe", (lambda e, kc=kc, ti=ti: e.matmul(PB[6][:, ti * NE:(ti + 1) * NE], lhsT=u2f[:, kc, :], rhs=wr_sb[:, kc, :],
                                                                                    start=(kc == 0), stop=(kc == KC - 1))), r=[u2f_d], w=[PBd[6]])
                                  chk("MA")
                                  T.op("act", lambda e: e.activation(out=rS[:], in_=PB[6][:, 0:MT * NE].rearrange("p (a b) -> p a b", b=NE), func=AF.Sigmoid),
                                       r=[PBd[6]], w=[rt_d])
                                  T.op("dve", lambda e: e.tensor_tensor(out=rSel[:], in0=rS[:], in1=brb_sb[:, 0:MT, :], op=ALU.add), r=[rt_d], w=[rt_d])
                                  selv = rSel[:].rearrange("p a (g k) -> p (a g) k", k=4)
                                  for pi_, (a_, b_) in enumerate(((0, 1), (0, 2), (0, 3), (1, 2), (1, 3), (2, 3))):
                                      T.op("dve", (lambda e, pi_=pi_, a_=a_, b_=b_: e.tensor_tensor(out=rP6[:, :, pi_], in0=selv[:, :, a_], in1=selv[:, :, b_], op=ALU.add)),
                                           r=[rt_d], w=[rt_d])
                                  T.op("dve", lambda e: e.reduce_max(out=rG[:].rearrange("p a g -> p (a g)"), in_=rP6[:], axis=AX.X), r=[rt_d], w=[rt_d])
                                  T.op("dve", lambda e: e.reduce_max(out=rGm[:], in_=rG[:], axis=AX.X), r=[rt_d], w=[rt_d])
                                  T.op("dve", lambda e: e.tensor_tensor(out=rG[:], in0=rG[:], in1=rGm[:].unsqueeze(2).to_broadcast([128, MT, 4]), op=ALU.is_ge),
                                       r=[rt_d], w=[rt_d])
                                  T.op("dve", lambda e: e.tensor_scalar(out=rPen[:], in0=rG[:], scalar1=-1.0, scalar2=1.0e4, op0=ALU.add, op1=ALU.mult),
                                       r=[rt_d], w=[rt_d])
                                  selv4 = rSel[:].rearrange("p a (g k) -> p a g k", k=4)
                                  T.op("dve", lambda e: e.tensor_tensor(out=selv4, in0=selv4, in1=rG[:].unsqueeze(3).to_broadcast([128, MT, 4, 4]), op=ALU.mult),
                                       r=[rt_d], w=[rt_d])
                                  T.op("dve", lambda e: e.tensor_tensor(out=selv4, in0=selv4, in1=rPen[:].unsqueeze(3).to_broadcast([128, MT, 4, 4]), op=ALU.add),
                                       r=[rt_d], w=[rt_d])
                                  for ti in range(MT):
                                      T.op("dve", (lambda e, ti=ti: e.max(out=rT8[:, ti, :], in_=rSel[:, ti, :])), r=[rt_d], w=[rt_d])
                                  T.op("dve", lambda e: e.tensor_tensor(out=rW[:], in0=rSel[:], in1=rT8[:, :, 1:2].to_broadcast([128, MT, NE]), op=ALU.is_ge),
                                       r=[rt_d], w=[rt_d])
                                  T.op("dve", lambda e: e.tensor_tensor(out=rW[:], in0=rW[:], in1=rS[:], op=ALU.mult), r=[rt_d], w=[rt_d])
                                  T.op("dve", lambda e: e.reduce_sum(out=rSum[:], in_=rW[:], axis=AX.X), r=[rt_d], w=[rt_d])
                                  T.op("dve", lambda e: e.reciprocal(out=rSum[:], in_=rSum[:]), r=[rt_d], w=[rt_d])
                                  T.op("dve", lambda e: e.tensor_tensor(out=rW[:], in0=rW[:], in1=rSum[:].unsqueeze(2).to_broadcast([128, MT, NE]), op=ALU.mult),
                                       r=[rt_d], w=[rt_d])
                                  if u == 0 and b == 0:
                                      dbg(f"rW{l}", rW[:], [rt_d])
                                  chk("MB")
                                  for ti in range(MT):
                                      pbk, pbd = PB[4 + ti // 4], PBd[4 + ti // 4]
                                      T.op("pe", (lambda e, ti=ti, pbk=pbk: e.transpose(out=pbk[0:16, (ti % 4) * 128:(ti % 4 + 1) * 128], in_=rW[:, ti, :], identity=identf[:])),
                                           r=[rt_d], w=[pbd])
                                  for hh in range((MT + 3) // 4):
                                      n_ = min(4, MT - 4 * hh) * 128
                                      T.op("act", (lambda e, hh=hh, n_=n_: e.activation(out=wT[:, hh * 512:hh * 512 + n_], in_=PB[4 + hh][0:16, 0:n_], func=AF.Copy)),
                                           r=[PBd[4 + hh]], w=[wT_d])
                                  T.op("dve", lambda e: e.memset(yacc[:], 0.0), w=[yacc_d])
                                  chk("MC")
                                  srcs = []
                                  for e_ in range(NE):
                                      for hf in range(2):
                                          srcs.append(lambda t, e_=e_, hf=hf: [(t[0][:], we1[l, e_, hf]), (t[1][:], we3[l, e_, hf]), (t[2][:], we2[l, e_, hf])])
                                  ering.begin(srcs)
                                  ering.issue_upto(1)
                                  steps = [(e_, hf, sg, m) for e_ in range(NE) for hf in range(2) for sg in range(NSG) for m in range(KC)]
                                  wts_ = {}

                                  def emitP(i):
                                      e_, hf, sg, m = steps[i]
                                      wbi = e_ % 2
                                      if hf == 0 and sg == 0 and m == 0:
                                          for hh in range(MU // 512):
                                              pbk, pbd = PB[6], PBd[6]
                                              T.op("dve", (lambda e, e_=e_, hh=hh: e.tensor_scalar(out=wTe[:], in0=wT[:, hh * 512:(hh + 1) * 512], scalar1=identf[0:16, e_:e_ + 1],
                                                                                                scalar2=None, op0=ALU.mult)), r=[wT_d], w=[wTe_d])
                                              T.op("pe", (lambda e: e.matmul(PB[6][:], lhsT=onesf[0:16, 0, :], rhs=wTe[:], start=True, stop=True)),
                                                   r=[wTe_d], w=[pbd])
                                              T.op("act", (lambda e, wbi=wbi, hh=hh: e.activation(out=wb_sb[wbi][:, hh * 512:(hh + 1) * 512], in_=PB[6][:], func=AF.Copy)),
                                                   r=[pbd], w=[wb_d[wbi]])
                                      if sg == 0 and m == 0:
                                          wts_[(e_, hf)] = ering.tile(e_ * 2 + hf)
                                      wt, wd = wts_[(e_, hf)]
                                      c0 = sg * 256
                                      si = i % 2
                                      pa, pad = PB[4 + si], PBd[4 + si]
                                      for kc in range(KC):
                                          T.op("pe", (lambda e, kc=kc: e.matmul(pa[:, 0:256], lhsT=wt[0][:, kc, m * 128:(m + 1) * 128],
                                                                               rhs=u2T[:, kc, c0:c0 + 256], start=(kc == 0), stop=(kc == KC - 1))),
                                               r=[wd, u2T_d], w=[pad])
                                      for kc in range(KC):
                                          T.op("pe", (lambda e, kc=kc: e.matmul(pa[:, 256:512], lhsT=wt[1][:, kc, m * 128:(m + 1) * 128],
                                                                               rhs=u2T[:, kc, c0:c0 + 256], start=(kc == 0), stop=(kc == KC - 1))),
                                               r=[wd, u2T_d], w=[pad])
                                      T.op("act", (lambda e: e.activation(out=silt[si][:], in_=pa[:, 0:256], func=AF.Silu)), r=[pad], w=[silt_d[si]])
                                      T.op("dve", (lambda e: e.tensor_tensor(out=silt[si][:], in0=silt[si][:], in1=wb_sb[wbi][:, c0:c0 + 256], op=ALU.mult)),
                                           r=[silt_d[si], wb_d[wbi]], w=[silt_d[si]])
                                      T.op("dve", (lambda e: e.tensor_tensor(out=hT[si][:], in0=pa[:, 256:512], in1=silt[si][:], op=ALU.mult)),
                                           r=[pad, silt_d[si]], w=[hT_d[si]])

                                  def emitY(i):
                                      e_, hf, sg, m = steps[i]
                                      wt, wd = wts_[(e_, hf)]
                                      si = i % 2
                                      for tt in range(2):
                                          for hv in range(2):
                                              yi = tt * 2 + hv
                                              T.op("pe", (lambda e, yi=yi, tt=tt, hv=hv: e.matmul(
                                                  PB[yi][:], lhsT=hT[si][:, tt * 128:(tt + 1) * 128], rhs=wt[2][:, m, hv * 512:(hv + 1) * 512],
                                                  start=(m == 0), stop=(m == KC - 1))), r=[wd, hT_d[si]], w=[PBd[yi]])
                                      if m == KC - 1:
                                          for tt in range(2):
                                              for hv in range(2):
                                                  yi = tt * 2 + hv
                                                  ti = sg * 2 + tt
                                                  T.op("dve", (lambda e, yi=yi, ti=ti, hv=hv: e.tensor_tensor(out=yacc[:, ti, hv * 512:(hv + 1) * 512],
                                                                                                            in0=PB[yi][:], in1=yacc[:, ti, hv * 512:(hv + 1) * 512], op=ALU.add)),
                                                       r=[PBd[yi], yacc_d], w=[yacc_d])

                                  emitP(0)
                                  for i in range(len(steps)):
                                      if i + 1 < len(steps):
                                          emitP(i + 1)
                                      emitY(i)
                                      if steps[i][2] == NSG - 1 and steps[i][3] == KC - 1:
                                          ering.issue_upto(steps[i][0] * 2 + steps[i][1] + 2)
                                  chk("MD")
                                  for ti in range(MT):
                                      xi = ti % 2
                                      T.dma("sp", f"xld{xi}", (lambda e, xi=xi, ti=ti: e.dma_start(out=xt[xi][:], in_=x1[b, u0 + ti * 128:u0 + (ti + 1) * 128, :])),
                                            w=[xt_d[xi]])
                                      T.op("dve", (lambda e, ti=ti: e.tensor_tensor(out=yacc[:, ti, :], in0=yacc[:, ti, :], in1=gb[:], op=ALU.mult)),
                                           r=[yacc_d, gbd], w=[yacc_d])
                                      T.op("dve", (lambda e, ti=ti, xi=xi: e.scalar_tensor_tensor(out=xt[xi][:], in0=xt[xi][:], scalar=DN_ALPHA, in1=yacc[:, ti, :],
                                                                                                op0=ALU.mult, op1=ALU.add)),
                                           r=[xt_d[xi], yacc_d], w=[xt_d[xi]])
                                      ln_rows(xt[xi][:], xt_d[xi], stm[:, xi, :], stm_d[xi])
                                      T.op("dve", (lambda e, xi=xi: e.tensor_scalar(out=xt[xi][:], in0=xt[xi][:], scalar1=stm[:, xi, 0:1], scalar2=stm[:, xi, 2:3],
                                                                                 op0=ALU.subtract, op1=ALU.mult)), r=[xt_d[xi], stm_d[xi]], w=[xt_d[xi]])
                                      T.op("dve", (lambda e, xi=xi: e.tensor_tensor(out=xt[xi][:], in0=xt[xi][:], in1=lnM[:, 0, :], op=ALU.mult)),
                                           r=[xt_d[xi], lnM_d], w=[xt_d[xi]])
                                      T.op("dve", (lambda e, xi=xi: e.tensor_tensor(out=xt[xi][:], in0=xt[xi][:], in1=lnM[:, 1, :], op=ALU.add)),
                                           r=[xt_d[xi], lnM_d], w=[xt_d[xi]])
                                      T.dma("sp", f"xst{xi}", (lambda e, xi=xi, ti=ti: e.dma_start(out=xdst[b, u0 + ti * 128:u0 + (ti + 1) * 128, :], in_=xt[xi][:])),
                                            r=[xt_d[xi]])
                              T.barrier()
                          T.barrier()
                  T.barrier()
              xsrc = xs[1]
        except _Stop:
            pass
        T.barrier()
    return nc, T.nins


def _consts(S):
    bf = ml_dtypes.bfloat16
    c = {}
    c["c_identb"] = np.eye(128, dtype=np.float32).astype(bf)
    c["c_identf"] = np.eye(128, dtype=np.float32)
    o = np.ones((128, 3, 128), np.float32)
    o[:, 1, :] = 1.0 / 512.0
    o[:, 2, :] = 1.0 / 128.0
    c["c_onesf"] = o
    P = np.zeros((128, 128), np.float32)
    for m in range(128):
        partner = m + 32 if (m % 64) < 32 else m - 32
        P[partner, m] = 1.0
    c["c_perm"] = P.astype(bf)
    half = 32
    inv = (10000.0 ** (-np.arange(half, dtype=np.float32) / half)).astype(np.float32)
    ang = np.arange(S, dtype=np.float32)[:, None] * inv[None, :]
    cos = np.cos(ang).T
    sin = np.sin(ang).T
    cs = np.zeros((128, 2, S), np.float32)
    for p in range(128):
        i = p % 32
        cs[p, 0] = cos[i]
        cs[p, 1] = -sin[i] if (p % 64) < 32 else sin[i]
    c["c_cs"] = cs.astype(bf)
    lg = np.log1p(-np.exp2(-5.0 - np.arange(4, dtype=np.float64)))
    dec = np.zeros((128, 20, 512), np.float64)
    ss = np.arange(128)[:, None]
    tt = np.arange(512)[None, :]
    for h in range(4):
        dec[:, h, :] = np.exp(lg[h] * (tt - ss))
        for jj in range(4):
            s = 128 * jj + ss
            cs_, ct_ = s // 64, tt // 64
            same = cs_ == ct_
            earlier = cs_ < ct_
            v = np.where(same, np.exp(lg[h] * np.abs(tt - s)), np.where(earlier, np.exp(lg[h] * (tt - s).clip(min=0)), 0.0))
            dec[:, 4 + 4 * h + jj, :] = v
    c["c_dec"] = dec.astype(np.float32).astype(bf)
    nm = np.zeros((128, 4, 512), np.float32)
    for jj in range(4):
        nm[:, jj, :] = np.where(128 * jj + ss <= tt, 0.0, NEG)
    c["c_nmask"] = nm.astype(bf)
    s8 = np.zeros((8, 8, 128), np.float32)
    for h in range(8):
        s8[h, h, :] = 1.0
    c["c_sel8"] = s8
    s16 = np.zeros((16, 16, 128), np.float32)
    for e in range(16):
        s16[e, e, :] = 1.0
    c["c_sel16"] = s16
    c["c_onesb"] = np.ones((128, 64), np.float32).astype(bf)
    c["c_gam"] = np.zeros((128, 64), np.float32)
    return c


def _kc_layout(w):
    sh = w.shape
    w = w.reshape(sh[:-2] + (KC, 128, sh[-1]))
    return np.ascontiguousarray(np.swapaxes(w, -3, -2))


def _prep_weights(inp, L):
    f = lambda a: np.asarray(a, dtype=np.float32)
    w = {}
    w_ada = f(inp["w_ada"])[:L]
    w["wada"] = np.ascontiguousarray(w_ada.reshape(L, KC, 128, 48, 128).transpose(0, 3, 2, 1, 4))
    w["bada"] = np.ascontiguousarray(f(inp["b_ada"])[:L].reshape(L, 48, 128).transpose(0, 2, 1))
    w_in = f(inp["w_in"])[:L]
    cols = []
    for c in range(4):
        cols.append(np.arange(A_GATE + c * 128, A_GATE + (c + 1) * 128))
        cols.append(np.arange(A_VAL + c * 128, A_VAL + (c + 1) * 128))
    for base, n in ((R_K, 2), (F_K, 4), (R_Q, 2), (F_Q, 4), (R_G, 4)):
        for c in range(n):
            cols.append(np.arange(base + c * 128, base + (c + 1) * 128))
    cols = np.stack(cols)
    wp = w_in[:, :, cols]
    w["wp"] = np.ascontiguousarray(wp.reshape(L, KC, 128, 24, 128).transpose(0, 3, 2, 1, 4))
    w["wf"] = _kc_layout(w_in[:, :, F_F:IN_COLS])
    w["wv"] = _kc_layout(np.concatenate([w_in[:, :, R_V:R_G], w_in[:, :, F_V:F_F]], axis=-1))
    wm = np.zeros((L, 8, 128, 36, 128), np.float32)
    for br, nm in enumerate(("w_conv_out", "w_ret_out", "w_fox_out")):
        wb_ = f(inp[nm])[:L].reshape(L, 4, 128, 8, 128)
        wm[:, :, :, br * 4:(br + 1) * 4, :] = wb_.transpose(0, 3, 2, 1, 4)
    wg = f(inp["w_gate"])[:L].reshape(L, KC, 128, 3, 8, 128)
    for br in range(3):
        wm[:, :, :, 12 + br * 8:12 + (br + 1) * 8, :] = wg[:, :, :, br].transpose(0, 3, 2, 1, 4)
    w["wm"] = wm
    w["bg"] = np.ascontiguousarray(f(inp["b_gate"])[:L].reshape(L, 24, 128).transpose(0, 2, 1))
    w["wo"] = _kc_layout(f(inp["w_out"])[:L])
    w["cw"] = np.ascontiguousarray(f(inp["conv_w"])[:L].reshape(L, CONV_K, 4, 128).transpose(0, 3, 2, 1))
    cpr = np.stack([f(inp["conv_b"])[:L], f(inp["conv_ln_g"])[:L], f(inp["conv_ln_b"])[:L]], axis=1)
    w["cprm"] = np.ascontiguousarray(cpr.reshape(L, 3, 4, 128).transpose(0, 3, 1, 2))
    w["bfg"] = np.ascontiguousarray(f(inp["b_forget"])[:L].reshape(L, 8, 1))
    lnp = np.stack([f(inp["ln1_g"])[:L], f(inp["ln1_b"])[:L], f(inp["ln2_g"])[:L], f(inp["ln2_b"])[:L]], axis=1)
    w["lnp"] = np.ascontiguousarray(np.broadcast_to(lnp[:, :, None, :], (L, 4, 128, D)))
    w["wr"] = _kc_layout(f(inp["w_router"]))
    w["brb"] = np.ascontiguousarray(np.broadcast_to(f(inp["b_router"])[None, None, :], (128, 8, NE)))
    w1 = f(inp["w1"])[:L].reshape(L, NE, KC, 128, 2, 1024)
    w["we1"] = np.ascontiguousarray(w1.transpose(0, 1, 4, 3, 2, 5))
    w3 = f(inp["w3"])[:L].reshape(L, NE, KC, 128, 2, 1024)
    w["we3"] = np.ascontiguousarray(w3.transpose(0, 1, 4, 3, 2, 5))
    w2 = f(inp["w2"])[:L].reshape(L, NE, 2, KC, 128, 1024)
    w["we2"] = np.ascontiguousarray(w2.transpose(0, 1, 2, 4, 3, 5))
    return w


def run(inputs, ncores, L, dump=None, trace=False, stop=None):
    x = np.asarray(inputs["x"], dtype=np.float32)
    c = np.asarray(inputs["c"], dtype=np.float32)
    B, S, _ = x.shape
    NSEQ = B // ncores
    nc, nins = build(NSEQ, S, L, dump, stop)
    shared = _prep_weights(inputs, L)
    shared.update(_consts(S))
    in_maps = []
    for i in range(ncores):
        m = dict(shared)
        m["x"] = np.ascontiguousarray(x[i * NSEQ:(i + 1) * NSEQ])
        ci = c[i * NSEQ:(i + 1) * NSEQ]
        m["ct"] = np.ascontiguousarray(ci.reshape(NSEQ, KC, 128).transpose(2, 1, 0))
        in_maps.append(m)
    res = run_bass_kernel_spmd(nc, in_maps, core_ids=list(range(ncores)), trace=trace)
    out = np.concatenate([r["out"] for r in res.results], axis=0)
    return out, res, nins


def kernel(**inputs):
    out, _, _ = run(inputs, NCORES, DEPTH)
    return out.astype(np.float32)
```

```python
import numpy as np
import ml_dtypes
from contextlib import ExitStack
import concourse.bass as bass
import concourse.mybir as mybir
from concourse.bass_utils import run_bass_kernel_spmd

F32, BF16 = mybir.dt.float32, mybir.dt.bfloat16
AF = mybir.ActivationFunctionType
ALU = mybir.AluOpType
AX = mybir.AxisListType

D = 1024
KC = 8
DEPTH = 2
CONV_CH, CONV_K = 512, 31
A_VAL, A_GATE, R_Q, R_K, R_V, R_G, F_Q, F_K, F_V, F_F, IN_COLS = 0, 512, 1024, 1280, 1536, 2048, 2560, 3072, 3584, 4096, 4104
NE, DFF = 16, 2048
DN_ALPHA = (2 * DEPTH) ** 0.25
EPS = 1e-5
NCORES = 8
NEG = -30000.0


class Dep:
    __slots__ = ("w", "r", "excl")

    def __init__(self, excl=False):
        self.w = None
        self.r = {}
        self.excl = excl


class Tr:
    def __init__(self, nc, es):
        self.nc, self.es = nc, es
        self.E = {"pe": nc.tensor, "act": nc.scalar, "dve": nc.vector, "pool": nc.gpsimd, "sp": nc.sync}
        self.sem, self.cnt = {}, {}
        for k in ("pe", "act", "dve", "pool"):
            self.newsem(k)
        self.known = {k: {} for k in self.E}
        self.nins = 0
        self.dead = False

    def newsem(self, key):
        self.sem[key] = self.es.enter_context(self.nc.semaphore("s_" + key))
        self.cnt[key] = 0

    def _deps(self, eng, r, w):
        need = {}
        for d in r:
            if d.w is not None:
                k, v = d.w
                if need.get(k, 0) < v:
                    need[k] = v
            if d.excl:
                for k, v in d.r.items():
                    if k != eng and need.get(k, 0) < v:
                        need[k] = v
        for d in w:
            if d.w is not None:
                k, v = d.w
                if need.get(k, 0) < v:
                    need[k] = v
            for k, v in d.r.items():
                if need.get(k, 0) < v:
                    need[k] = v
        kn = self.known[eng]
        for k, v in need.items():
            if k == "pe" and eng == "pe":
                continue
            if kn.get(k, 0) >= v:
                continue
            self.E[eng].wait_ge(self.sem[k], v)
            kn[k] = v

    def op(self, eng, fn, r=(), w=()):
        if self.dead:
            return
        self._deps(eng, r, w)
        ins = fn(self.E[eng])
        self.cnt[eng] += 1
        c = self.cnt[eng]
        ins.then_inc(self.sem[eng], 1)
        self.nins += 1
        for d in r:
            d.r[eng] = c
        for d in w:
            d.w = (eng, c)
            d.r = {}

    def dma(self, q, key, fn, r=(), w=()):
        if self.dead:
            return
        self._deps(q, r, w)
        ins = fn(self.E[q])
        self.cnt[key] += 16
        c = self.cnt[key]
        ins.then_inc(self.sem[key], 16)
        self.nins += 1
        for d in r:
            d.r[key] = c
        for d in w:
            d.w = (key, c)
            d.r = {}

    def barrier(self):
        if self.dead:
            return
        for eng in self.E:
            kn = self.known[eng]
            for k, c in self.cnt.items():
                if c > 0 and not (k == "pe" and eng == "pe") and kn.get(k, 0) < c:
                    self.E[eng].wait_ge(self.sem[k], c)
                    kn[k] = c


class Ring:
    def __init__(self, T, es, name, shapes, dtype, n, q, semname=None):
        self.T, self.n, self.q = T, n, q
        self.tiles = [[es.enter_context(T.nc.sbuf_tensor(f"{name}{i}_{j}", sh, dtype)) for j, sh in enumerate(shapes)]
                      for i in range(n)]
        self.deps = [Dep() for _ in range(n)]
        self.keys = [f"{semname or name}{i}" for i in range(n)]
        for k in self.keys:
            if k not in T.sem:
                T.newsem(k)
        self.pos = 0
        self.srcs = []
        self.base = 0
        self.issued = 0

    def begin(self, srcs):
        self.pos += len(self.srcs)
        self.base = self.pos
        self.srcs = srcs
        self.issued = 0

    def _issue(self, j):
        s = (self.base + j) % self.n
        pairs = self.srcs[j](self.tiles[s])
        for (o, i_) in pairs:
            self.T.dma(self.q, self.keys[s], (lambda e, o=o, i_=i_: e.dma_start(out=o, in_=i_)), w=[self.deps[s]])

    def issue_upto(self, j):
        j = min(j, len(self.srcs) - 1)
        while self.issued <= j:
            self._issue(self.issued)
            self.issued += 1

    def tile(self, i):
        assert i < self.issued
        s = (self.base + i) % self.n
        return self.tiles[s], self.deps[s]

    def get(self, i):
        lim = min(i + self.n - 1, len(self.srcs) - 1)
        while self.issued <= lim:
            self._issue(self.issued)
            self.issued += 1
        s = (self.base + i) % self.n
        return self.tiles[s], self.deps[s]


class _Stop(Exception):
    pass


def build(NSEQ, S, L, dump=None, stop=None):
    NT = S // 128
    NG = S // 512
    MU = min(S, 1024)
    NU = S // MU
    MT = MU // 128
    NSG = MU // 256
    nc = bass.Bass("TRN2", target_bir_lowering=False)

    def din(name, shape, dt=F32):
        return nc.dram_tensor(name, list(shape), dt, kind="ExternalInput").ap()

    x_in = din("x", [NSEQ, S, D])
    ct_in = din("ct", [128, KC, NSEQ])
    wada = din("wada", [L, 48, 128, KC, 128])
    bada = din("bada", [L, 128, 48])
    wp = din("wp", [L, 24, 128, KC, 128])
    wf = din("wf", [L, 128, KC, 8])
    wv = din("wv", [L, 128, KC, 1024])
    wm = din("wm", [L, 8, 128, 36, 128])
    bg = din("bg", [L, 128, 24])
    wo = din("wo", [L, 128, KC, 1024])
    cw = din("cw", [L, 128, 4, CONV_K])
    cprm = din("cprm", [L, 128, 3, 4])
    bfg = din("bfg", [L, 8, 1])
    lnp = din("lnp", [L, 4, 128, D])
    wr = din("wr", [128, KC, NE])
    brb = din("brb", [128, 8, NE])
    we1 = din("we1", [L, NE, 2, 128, KC, 1024])
    we3 = din("we3", [L, NE, 2, 128, KC, 1024])
    we2 = din("we2", [L, NE, 2, 128, KC, 1024])
    c_identb = din("c_identb", [128, 128], BF16)
    c_identf = din("c_identf", [128, 128])
    c_onesf = din("c_onesf", [128, 3, 128])
    c_perm = din("c_perm", [128, 128], BF16)
    c_cs = din("c_cs", [128, 2, S], BF16)
    c_dec = din("c_dec", [128, 7168], BF16)
    c_nmask = din("c_nmask", [128, 1280], BF16)
    c_sel8 = din("c_sel8", [8, 8, 128])
    c_sel16 = din("c_sel16", [16, 16, 128])
    c_onesb = din("c_onesb", [128, 64], BF16)
    c_gam = din("c_gam", [128, 64])
    out_d = nc.dram_tensor("out", [NSEQ, S, D], F32, kind="ExternalOutput").ap()
    xs = [nc.dram_tensor(f"xscr{i}", [NSEQ, S, D], F32, kind="Internal").ap() for i in range(2)]
    dumps = {}
    if dump:
        for nm, (shape, dt) in dump.items():
            dumps[nm] = nc.dram_tensor("dbg_" + nm, list(shape), dt, kind="ExternalOutput").ap()

    log_gamma = [float(np.log1p(-np.exp2(-5.0 - h))) for h in range(4)]
    DOFF = [0, 512, 896, 1152]

    with ExitStack() as es:
        T = Tr(nc, es)
        for k in ("const", "dbg", "decs", "wfs", "css", "lnps", "xld0", "xld1", "xst0", "xst1"):
            T.newsem(k)

        def sb(name, shape, dt=F32, stack=es):
            return stack.enter_context(nc.sbuf_tensor(name, list(shape), dt))

        PB = [es.enter_context(nc.psum_tensor(f"pb{i}", [128, 512], F32)) for i in range(7)]
        PBd = [Dep(True) for _ in range(7)]
        PT = es.enter_context(nc.psum_tensor("ptb", [128, 1024], BF16))
        PTd = Dep(True)

        identb = sb("identb", [128, 128], BF16)
        identf = sb("identf", [128, 128])
        onesf = sb("onesf", [128, 3, 128])
        permR = sb("permR", [128, 128], BF16)
        onesb = sb("onesb", [128, 64], BF16)
        ones8 = sb("ones8", [8, 512])
        siluc = sb("siluc", [128, KC, NSEQ])
        wr_sb = sb("wr_sb", [128, KC, NE])
        brb_sb = sb("brb_sb", [128, 8, NE])
        cdep = Dep()
        for (o, i_) in ((identb, c_identb), (identf, c_identf), (onesf, c_onesf), (permR, c_perm), (onesb, c_onesb),
                        (siluc, ct_in), (wr_sb, wr), (brb_sb, brb)):
            T.dma("sp", "const", (lambda e, o=o, i_=i_: e.dma_start(out=o[:], in_=i_)), w=[cdep])
        T.op("dve", lambda e: e.memset(ones8[:], 1.0), w=[cdep])
        T.op("act", lambda e: e.activation(out=siluc[:], in_=siluc[:], func=AF.Silu), r=[cdep], w=[cdep])
        T.barrier()
        cdep = Dep()

        def dbg(name, ap, deps):
            if name in dumps:
                T.dma("sp", "dbg", (lambda e: e.dma_start(out=dumps[name], in_=ap)), r=deps)

        def rstd_from_var(var_ap, out_ap, tmp_dep, shape_cols):
            T.op("act", lambda e: e.activation(out=out_ap, in_=var_ap, func=AF.Ln, bias=EPS, scale=1.0), r=[tmp_dep], w=[tmp_dep])
            T.op("act", lambda e: e.activation(out=out_ap, in_=out_ap, func=AF.Exp, scale=-0.5), r=[tmp_dep], w=[tmp_dep])

        def ln_rows(xt, xdep, st_t, st_dep):
            T.op("dve", lambda e: e.bn_stats(out=st_t[:, 4:10], in_=xt[:, 0:512]), r=[xdep], w=[st_dep])
            T.op("dve", lambda e: e.bn_stats(out=st_t[:, 10:16], in_=xt[:, 512:1024]), r=[xdep], w=[st_dep])
            T.op("dve", lambda e: e.bn_aggr(out=st_t[:, 0:2], in_=st_t[:, 4:16]), r=[st_dep], w=[st_dep])
            rstd_from_var(st_t[:, 1:2], st_t[:, 2:3], st_dep, 1)

        def chk(tag):
            if stop == tag and not T.dead:
                T.barrier()
                T.dead = True

        xsrc = x_in
        try:
          for l in range(L):
              last_layer = (l == L - 1)
              x1 = xs[0]
              xdst = out_d if last_layer else xs[1]
              with ExitStack() as ls:
                  adaT = sb(f"adaT{l}", [128, 48, NSEQ], F32, ls)
                  bada_sb = sb(f"bada{l}", [128, 48], F32, ls)
                  cw_sb = sb(f"cw{l}", [128, 4, CONV_K], F32, ls)
                  cprm_sb = sb(f"cprm{l}", [128, 3, 4], F32, ls)
                  bfg_sb = sb(f"bfg{l}", [8, 1], F32, ls)
                  bg_sb = sb(f"bg{l}", [128, 24], F32, ls)
                  ldep = Dep()
                  T.dma("sp", "const", lambda e: e.dma_start(out=bada_sb[:], in_=bada[l]), w=[ldep])
                  T.dma("sp", "const", lambda e: e.dma_start(out=cw_sb[:], in_=cw[l]), w=[ldep])
                  T.dma("sp", "const", lambda e: e.dma_start(out=cprm_sb[:], in_=cprm[l]), w=[ldep])
                  T.dma("sp", "const", lambda e: e.dma_start(out=bfg_sb[:], in_=bfg[l]), w=[ldep])
                  T.dma("sp", "const", lambda e: e.dma_start(out=bg_sb[:], in_=bg[l]), w=[ldep])
                  T.op("dve", lambda e: e.tensor_scalar(out=bfg_sb[:], in0=bfg_sb[:], scalar1=-1.0, scalar2=None, op0=ALU.mult),
                       r=[ldep], w=[ldep])
                  with ExitStack() as ps_:
                      aring = Ring(T, ps_, f"ada{l}_", [[128, KC, 128]], F32, 3, "sp", "ada")
                      aring.begin([(lambda t, ch=ch: [(t[0][:], wada[l, ch])]) for ch in range(48)])
                      for ch in range(48):
                          wt, wd = aring.get(ch)
                          pb, pd = PB[ch % 2], PBd[ch % 2]
                          for kc in range(KC):
                              T.op("pe", (lambda e, kc=kc, wt=wt, pb=pb: e.matmul(pb[:, 0:NSEQ], lhsT=wt[0][:, kc, :], rhs=siluc[:, kc, :],
                                                                               start=(kc == 0), stop=(kc == KC - 1))),
                                   r=[wd], w=[pd])
                          one = 1.0 if (8 <= ch < 16 or 32 <= ch < 40) else 0.0
                          T.op("dve", (lambda e, ch=ch, pb=pb, one=one: e.tensor_scalar(out=adaT[:, ch, :], in0=pb[:, 0:NSEQ],
                                                                                     scalar1=bada_sb[:, ch:ch + 1], scalar2=one,
                                                                                     op0=ALU.add, op1=ALU.add)),
                               r=[pd, ldep], w=[ldep])
                      T.barrier()
                  T.barrier()

                  def build_gb(gb, gtmp, base, b, gbd, gtd):
                      for hh in range(2):
                          pb, pd = PB[hh], PBd[hh]
                          for c4 in range(4):
                              ch = hh * 4 + c4
                              T.op("dve", (lambda e, ch=ch: e.tensor_scalar(
                                  out=gtmp[:], in0=onesf[:, 0, :], scalar1=adaT[:, base + ch, b:b + 1], scalar2=None, op0=ALU.mult)),
                                  r=[ldep], w=[gtd])
                              T.op("pe", (lambda e, c4=c4, pb=pb: e.matmul(pb[:, c4 * 128:(c4 + 1) * 128], lhsT=gtmp[:], rhs=identf[:],
                                                                        start=True, stop=True)), r=[gtd], w=[pd])
                          T.op("act", (lambda e, hh=hh, pb=pb: e.activation(out=gb[:, hh * 512:(hh + 1) * 512], in_=pb[:], func=AF.Copy)),
                               r=[pd], w=[gbd])

                  chk("ada")
                  for b in range(NSEQ):
                      if True:
                          with ExitStack() as as_:
                              gb = sb(f"gba{l}_{b}", [128, D], F32, as_)
                              gtmp = sb(f"gtmpa{l}_{b}", [128, 128], F32, as_)
                              lnA = sb(f"lnA{l}_{b}", [128, 2, D], F32, as_)
                              dec = sb(f"dec{l}_{b}", [128, 7168], BF16, as_)
                              nmask = sb(f"nmask{l}_{b}", [128, 1280], BF16, as_)
                              rk = sb(f"rk{l}_{b}", [128, 2, S], BF16, as_)
                              fk = sb(f"fk{l}_{b}", [128, 4, S], BF16, as_)
                              rv = sb(f"rv{l}_{b}", [128, NT, 512], BF16, as_)
                              fv = sb(f"fv{l}_{b}", [128, NT, 512], BF16, as_)
                              ncumT = sb(f"ncumT{l}_{b}", [8, 512], F32, as_)
                              carry = sb(f"carry{l}_{b}", [8, 1], F32, as_)
                              ncumK = sb(f"ncumK{l}_{b}", [128, NT, 8], F32, as_)
                              hbuf = sb(f"hbuf{l}_{b}", [128, 4, 30 + 512], BF16, as_)
                              cs = sb(f"cs{l}_{b}", [128, 2, 512], BF16, as_)
                              uT = sb(f"uT{l}_{b}", [128, KC, 512], BF16, as_)
                              xg = [sb(f"xg{l}_{b}_{i}", [128, D], F32, as_) for i in range(2)]
                              xnb = sb(f"xnb{l}_{b}", [128, D], BF16, as_)
                              stt = sb(f"stt{l}_{b}", [128, 2, 16], F32, as_)
                              sig = sb(f"sig{l}_{b}", [128, 512], BF16, as_)
                              rt1 = sb(f"rt1{l}_{b}", [128, 512], F32, as_)
                              rt2 = sb(f"rt2{l}_{b}", [128, 512], F32, as_)
                              rq = sb(f"rq{l}_{b}", [128, 2, 512], BF16, as_)
                              fq = sb(f"fq{l}_{b}", [128, 4, 512], BF16, as_)
                              rgs = sb(f"rgs{l}_{b}", [128, 4, 512], BF16, as_)
                              yy = sb(f"yy{l}_{b}", [128, 12, 512], BF16, as_)
                              acc = sb(f"acc{l}_{b}", [128, 4, 512], F32, as_)
                              mrg = sb(f"mrg{l}_{b}", [128, KC, 512], BF16, as_)
                              mean_sb = sb(f"mean{l}_{b}", [128, 512], F32, as_)
                              rstd_sb = sb(f"rstd{l}_{b}", [128, 512], F32, as_)
                              pT = [sb(f"pT{l}_{b}_{i}", [128, 512], BF16, as_) for i in range(2)]
                              cq = sb(f"cq{l}_{b}", [128, 512], F32, as_)
                              lft = rt2[0:8, :]
                              raw = sig
                              cm8 = rt2[0:8, :]
                              sq = rt2
                              macc = rt1
                              sgt = cq
                              tmpf = [mean_sb, rstd_sb]
                              pring = Ring(T, as_, f"pr{l}_{b}_", [[128, KC, 128]], BF16, 3, "pool", "pr")
                              mring = Ring(T, as_, f"mr{l}_{b}_", [[128, 36, 128]], BF16, 2, "pool", "mr")
                              bigw = sb(f"bigw{l}_{b}", [128, KC, 512], BF16, as_)
                              wf_sb = sb(f"wf{l}_{b}", [128, KC, 8], BF16, as_)
                              bigk = "big"
                              if bigk not in T.sem:
                                  T.newsem(bigk)
                              (dec_d, hb_d, cs_d, uT_d, xnb_d, sig_d, raw_d0, rt_d, rq_d, fq_d, rgs_d, acc_d, mean_d, rstd_d, cq_d, mrg_d,
                               big_d, wf_d, gbd, gtd, lnA_d, ncT_d, car_d, cm8_d0) = [Dep() for _ in range(24)]
                              lft_d = rt_d
                              raw_d = sig_d
                              cm8_d = rt_d
                              sq_d = rt_d
                              macc_d = rt_d
                              sgt_d = cq_d
                              tmpf_d = [mean_d, rstd_d]
                              xg_d = [Dep(), Dep()]
                              st_d = [Dep(), Dep()]
                              yy_d = [Dep() for _ in range(12)]
                              pT_d = [Dep(), Dep()]
                              rk_g = [Dep() for _ in range(NG)]
                              fk_g = [Dep() for _ in range(NG)]
                              rv_g = [Dep() for _ in range(NG)]
                              fv_g = [Dep() for _ in range(NG)]
                              ncK_g = [Dep() for _ in range(NG)]
                              T.dma("sp", "decs", lambda e: e.dma_start(out=dec[:], in_=c_dec), w=[dec_d])
                              T.dma("sp", "decs", lambda e: e.dma_start(out=nmask[:], in_=c_nmask), w=[dec_d])
                              T.dma("pool", "wfs", lambda e: e.dma_start(out=wf_sb[:], in_=wf[l]), w=[wf_d])
                              for j in range(2):
                                  T.dma("sp", "lnps", (lambda e, j=j: e.dma_start(out=lnA[:, j, :], in_=lnp[l, j])), w=[lnA_d])
                              T.op("dve", lambda e: e.memset(hbuf[:, :, 0:30], 0.0), w=[hb_d])
                              build_gb(gb, gtmp, 16, b, gbd, gtd)
                              pp_i = [0]

                              def pp():
                                  i = pp_i[0] % 2
                                  pp_i[0] += 1
                                  return PB[i], PBd[i]

                              sc_i = [0]

                              def scb():
                                  i = 2 + sc_i[0] % 2
                                  sc_i[0] += 1
                                  return PB[i], PBd[i]

                              xcnt = [0]

                              def xload(src_ap):
                                  i = xcnt[0] % 2
                                  xcnt[0] += 1
                                  T.dma("sp", f"xld{i}", (lambda e: e.dma_start(out=xg[i][:], in_=src_ap)), w=[xg_d[i]])
                                  return i

                              for g in range(NG):
                                  t0 = g * 512
                                  pring.begin([(lambda t, ci=ci: [(t[0][:], wp[l, ci])]) for ci in range(24)])
                                  pring.issue_upto(2)
                                  T.dma("pool", bigk, lambda e: e.dma_start(out=bigw[:], in_=wv[l, :, :, 0:512]), w=[big_d])
                                  T.dma("sp", "css", (lambda e, t0=t0: e.dma_start(out=cs[:], in_=c_cs[:, :, t0:t0 + 512])), w=[cs_d])
                                  for tt in range(4):
                                      xi = xload(xsrc[b, t0 + tt * 128:t0 + (tt + 1) * 128, :])
                                      ln_rows(xg[xi][:], xg_d[xi], stt[:, xi, :], st_d[xi])
                                      T.op("dve", (lambda e, xi=xi: e.tensor_scalar(out=xnb[:], in0=xg[xi][:], scalar1=stt[:, xi, 0:1],
                                                                                 scalar2=stt[:, xi, 2:3], op0=ALU.subtract, op1=ALU.mult)),
                                           r=[xg_d[xi], st_d[xi]], w=[xnb_d])
                                      for kc in range(KC):
                                          T.op("pe", (lambda e, kc=kc: e.transpose(out=PT[:, kc * 128:(kc + 1) * 128], in_=xnb[:, kc * 128:(kc + 1) * 128],
                                                                                   identity=identb[:])), r=[xnb_d], w=[PTd])
                                      for kc in range(KC):
                                          T.op("act", (lambda e, kc=kc, tt=tt: e.activation(out=uT[:, kc, tt * 128:(tt + 1) * 128],
                                                                                          in_=PT[:, kc * 128:(kc + 1) * 128], func=AF.Identity,
                                                                                          bias=adaT[:, kc, b:b + 1], scale=adaT[:, 8 + kc, b:b + 1])),
                                               r=[PTd, ldep], w=[uT_d])
                                  if g == 0 and b == 0:
                                      dbg(f"uT{l}", uT[:], [uT_d])

                                  chk("A")
                                  def proj(ci):
                                      wt, wd = pring.get(ci)
                                      pb, pd = pp()
                                      for kc in range(KC):
                                          T.op("pe", (lambda e, kc=kc, wt=wt, pb=pb: e.matmul(pb[:], lhsT=wt[0][:, kc, :], rhs=uT[:, kc, :],
                                                                                           start=(kc == 0), stop=(kc == KC - 1))),
                                               r=[wd, uT_d], w=[pd])
                                      return pb, pd

                                  def rotary(pb, pd, dst_ap, dst_deps):
                                      T.op("act", lambda e: e.activation(out=raw[:], in_=pb[:], func=AF.Copy), r=[pd], w=[raw_d])
                                      p2, p2d = pp()
                                      T.op("pe", lambda e: e.matmul(p2[:], lhsT=permR[:], rhs=raw[:], start=True, stop=True), r=[raw_d], w=[p2d])
                                      T.op("act", lambda e: e.activation(out=rt1[:], in_=pb[:], func=AF.Copy), r=[pd], w=[rt_d])
                                      T.op("act", lambda e: e.activation(out=rt2[:], in_=p2[:], func=AF.Copy), r=[p2d], w=[rt_d])
                                      T.op("dve", lambda e: e.tensor_tensor(out=rt1[:], in0=rt1[:], in1=cs[:, 0, :], op=ALU.mult), r=[rt_d, cs_d], w=[rt_d])
                                      T.op("dve", lambda e: e.tensor_tensor(out=rt2[:], in0=rt2[:], in1=cs[:, 1, :], op=ALU.mult), r=[rt_d, cs_d], w=[rt_d])
                                      T.op("dve", lambda e: e.tensor_tensor(out=dst_ap, in0=rt1[:], in1=rt2[:], op=ALU.add), r=[rt_d], w=dst_deps)

                                  for c in range(4):
                                      pb, pd = proj(2 * c)
                                      T.op("act", (lambda e, pb=pb: e.activation(out=sig[:], in_=pb[:], func=AF.Sigmoid)), r=[pd], w=[sig_d])
                                      pb, pd = proj(2 * c + 1)
                                      T.op("dve", (lambda e, pb=pb, c=c: e.tensor_tensor(out=hbuf[:, c, 30:542], in0=pb[:], in1=sig[:], op=ALU.mult)),
                                           r=[pd, sig_d], w=[hb_d])
                                  chk("B1")
                                  for c in range(2):
                                      pb, pd = proj(8 + c)
                                      rotary(pb, pd, rk[:, c, t0:t0 + 512], [rk_g[g]])
                                  chk("B2")
                                  for c in range(4):
                                      pb, pd = proj(10 + c)
                                      T.op("act", (lambda e, pb=pb, c=c: e.activation(out=fk[:, c, t0:t0 + 512], in_=pb[:], func=AF.Copy)), r=[pd], w=[fk_g[g]])
                                  for c in range(2):
                                      pb, pd = proj(14 + c)
                                      rotary(pb, pd, rq[:, c, :], [rq_d])
                                  for c in range(4):
                                      pb, pd = proj(16 + c)
                                      T.op("act", (lambda e, pb=pb, c=c: e.activation(out=fq[:, c, :], in_=pb[:], func=AF.Copy)), r=[pd], w=[fq_d])
                                  for c in range(4):
                                      pb, pd = proj(20 + c)
                                      T.op("act", (lambda e, pb=pb, c=c: e.activation(out=rgs[:, c, :], in_=pb[:], func=AF.Silu)), r=[pd], w=[rgs_d])
                                  chk("B3")
                                  pb, pd = pp()
                                  for kc in range(KC):
                                      T.op("pe", (lambda e, kc=kc, pb=pb: e.matmul(pb[0:8, :], lhsT=wf_sb[:, kc, :], rhs=uT[:, kc, :],
                                                                                start=(kc == 0), stop=(kc == KC - 1))), r=[wf_d, uT_d], w=[pd])
                                  T.op("act", (lambda e, pb=pb: e.activation(out=lft, in_=pb[0:8, :], func=AF.Exp, bias=bfg_sb[:, 0:1], scale=-1.0)),
                                       r=[pd, ldep], w=[lft_d])
                                  T.op("act", lambda e: e.activation(out=lft, in_=lft, func=AF.Ln, bias=1.0, scale=1.0), r=[lft_d], w=[lft_d])
                                  if g > 0:
                                      T.op("act", lambda e: e.activation(out=carry[:], in_=ncumT[:, 511:512], func=AF.Copy), r=[ncT_d], w=[car_d])
                                  init = 0.0 if g == 0 else carry[:, 0:1]
                                  T.op("dve", (lambda e, init=init: e.tensor_tensor_scan(out=ncumT[:], data0=ones8[:], data1=lft,
                                                                                      initial=init, op0=ALU.mult, op1=ALU.add)),
                                       r=[lft_d, car_d], w=[ncT_d])
                                  pb, pd = pp()
                                  for tt in range(4):
                                      T.op("pe", (lambda e, tt=tt, pb=pb: e.transpose(out=pb[:, tt * 8:(tt + 1) * 8],
                                                                                   in_=ncumT[:, tt * 128:(tt + 1) * 128],
                                                                                   identity=identf[0:8, 0:8])), r=[ncT_d], w=[pd])
                                  T.op("act", (lambda e, pb=pb, g=g: e.activation(out=ncumK[:, 4 * g:4 * g + 4, :],
                                                                               in_=pb[:, 0:32].rearrange("p (a b) -> p a b", b=8), func=AF.Copy)),
                                       r=[pd], w=[ncK_g[g]])
                                  chk("B4")
                                  for hv in range(2):
                                      if hv == 1:
                                          T.dma("pool", bigk, (lambda e, hv=hv: e.dma_start(out=bigw[:], in_=wv[l, :, :, hv * 512:(hv + 1) * 512])), w=[big_d])
                                      for tt in range(4):
                                          pb, pd = pp()
                                          for kc in range(KC):
                                              T.op("pe", (lambda e, kc=kc, pb=pb, tt=tt: e.matmul(
                                                  pb[:], lhsT=uT[:, kc, tt * 128:(tt + 1) * 128], rhs=bigw[:, kc, :],
                                                  start=(kc == 0), stop=(kc == KC - 1))), r=[big_d, uT_d], w=[pd])
                                          dst = rv if hv == 0 else fv
                                          dd = rv_g[g] if hv == 0 else fv_g[g]
                                          T.op("act", (lambda e, pb=pb, dst=dst, tt=tt, g=g: e.activation(out=dst[:, 4 * g + tt, :], in_=pb[:], func=AF.Copy)),
                                               r=[pd], w=[dd])

                                  chk("B")
                                  for c in range(4):
                                      T.op("dve", (lambda e, c=c: e.tensor_scalar(out=acc[:, c, :], in0=hbuf[:, c, 0:512], scalar1=cw_sb[:, c, 0:1],
                                                                               scalar2=cprm_sb[:, 0, c:c + 1], op0=ALU.mult, op1=ALU.add)),
                                           r=[hb_d, ldep], w=[acc_d])
                                  for k in range(1, CONV_K):
                                      for c in range(4):
                                          T.op("dve", (lambda e, c=c, k=k: e.scalar_tensor_tensor(out=acc[:, c, :], in0=hbuf[:, c, k:k + 512],
                                                                                                scalar=cw_sb[:, c, k:k + 1], in1=acc[:, c, :],
                                                                                                op0=ALU.mult, op1=ALU.add)),
                                               r=[hb_d, acc_d], w=[acc_d])
                                  for c in range(4):
                                      T.op("act", (lambda e, c=c: e.activation(out=hbuf[:, c, 0:30], in_=hbuf[:, c, 512:542], func=AF.Copy)),
                                           r=[hb_d], w=[hb_d])

                                  def ln_feat(chunks, cdeps, ones_ap, nch):
                                      pm, pmd = pp()
                                      for i, ch in enumerate(chunks):
                                          T.op("pe", (lambda e, i=i, ch=ch: e.matmul(pm[:], lhsT=ones_ap, rhs=ch, start=(i == 0), stop=(i == nch - 1))),
                                               r=cdeps, w=[pmd])
                                      pe2, pe2d = pp()
                                      for i, ch in enumerate(chunks):
                                          T.op("act", (lambda e, ch=ch: e.activation(out=sq[:], in_=ch, func=AF.Square)), r=cdeps, w=[sq_d])
                                          T.op("pe", (lambda e, i=i: e.matmul(pe2[:], lhsT=ones_ap, rhs=sq[:], start=(i == 0), stop=(i == nch - 1))),
                                               r=[sq_d], w=[pe2d])
                                      T.op("act", lambda e: e.activation(out=mean_sb[:], in_=pm[:], func=AF.Copy), r=[pmd], w=[mean_d])
                                      T.op("dve", lambda e: e.tensor_tensor(out=rstd_sb[:], in0=mean_sb[:], in1=mean_sb[:], op=ALU.mult), r=[mean_d], w=[rstd_d])
                                      T.op("dve", lambda e: e.tensor_tensor(out=rstd_sb[:], in0=pe2[:], in1=rstd_sb[:], op=ALU.subtract), r=[pe2d, rstd_d], w=[rstd_d])
                                      rstd_from_var(rstd_sb[:], rstd_sb[:], rstd_d, 512)

                                  ln_feat([acc[:, c, :] for c in range(4)], [acc_d], onesf[:, 1, :], 4)
                                  for c in range(4):
                                      T.op("dve", (lambda e, c=c: e.tensor_tensor(out=acc[:, c, :], in0=acc[:, c, :], in1=mean_sb[:], op=ALU.subtract)),
                                           r=[acc_d, mean_d], w=[acc_d])
                                      T.op("dve", (lambda e, c=c: e.tensor_tensor(out=acc[:, c, :], in0=acc[:, c, :], in1=rstd_sb[:], op=ALU.mult)),
                                           r=[acc_d, rstd_d], w=[acc_d])
                                      T.op("act", (lambda e, c=c: e.activation(out=yy[:, c, :], in_=acc[:, c, :], func=AF.Silu,
                                                                            bias=cprm_sb[:, 2, c:c + 1], scale=cprm_sb[:, 1, c:c + 1])),
                                           r=[acc_d, ldep], w=[yy_d[c]])

                                  chk("C")
                                  nkt = 4 * g + 4
                                  for h in range(4):
                                      c, r0 = h // 2, (h % 2) * 64
                                      po, pod = PB[4], PBd[4]
                                      def retS(j):
                                          ps, psd = scb()
                                          q0 = 128 * max(0, j - 4 * g)
                                          T.op("pe", (lambda e: e.matmul(ps[:, q0:512], lhsT=rk[r0:r0 + 64, c, j * 128:(j + 1) * 128],
                                                                        rhs=rq[r0:r0 + 64, c, q0:512], start=True, stop=True)),
                                               r=[rk_g[j // 4], rq_d], w=[psd])
                                          pi = j % 2
                                          if j < 4 * g:
                                              sc = 0.125 * float(np.exp(log_gamma[h] * 128.0 * (4 * g - j)))
                                              dt_ = dec[:, h * 512:(h + 1) * 512]
                                          else:
                                              sc = 0.125
                                              jj = j - 4 * g
                                              o_ = 2048 + h * 1280 + DOFF[jj]
                                              dt_ = dec[:, o_:o_ + 512 - 128 * jj]
                                          T.op("dve", (lambda e: e.scalar_tensor_tensor(out=pT[pi][:, q0:512], in0=ps[:, q0:512], scalar=sc, in1=dt_,
                                                                                       op0=ALU.mult, op1=ALU.mult)),
                                               r=[psd, dec_d], w=[pT_d[pi]])

                                      def retV(j):
                                          pi = j % 2
                                          q0 = 128 * max(0, j - 4 * g)
                                          T.op("pe", (lambda e: e.matmul(po[:, q0:512], lhsT=rv[:, j, h * 128:(h + 1) * 128], rhs=pT[pi][:, q0:512],
                                                                        start=(j == 0), stop=(j == nkt - 1))),
                                               r=[rv_g[j // 4], pT_d[pi]], w=[pod])

                                      for j in range(nkt):
                                          retS(j)
                                          if j > 0:
                                              retV(j - 1)
                                      retV(nkt - 1)
                                      T.op("act", lambda e: e.activation(out=acc[:, 0, :], in_=po[:], func=AF.Copy), r=[pod], w=[acc_d])
                                      ln_feat([acc[:, 0, :]], [acc_d], onesf[:, 2, :], 1)
                                      T.op("dve", lambda e: e.tensor_tensor(out=acc[:, 0, :], in0=acc[:, 0, :], in1=mean_sb[:], op=ALU.subtract),
                                           r=[acc_d, mean_d], w=[acc_d])
                                      T.op("dve", lambda e: e.tensor_tensor(out=acc[:, 0, :], in0=acc[:, 0, :], in1=rstd_sb[:], op=ALU.mult),
                                           r=[acc_d, rstd_d], w=[acc_d])
                                      T.op("dve", (lambda e, h=h: e.tensor_tensor(out=yy[:, 4 + h, :], in0=acc[:, 0, :], in1=rgs[:, h, :], op=ALU.mult)),
                                           r=[acc_d, rgs_d], w=[yy_d[4 + h]])

                                  chk("D")
                                  mring.begin([(lambda t, dc=dc: [(t[0][:], wm[l, dc])]) for dc in range(8)])
                                  mring.issue_upto(1)
                                  T.dma("pool", bigk, lambda e: e.dma_start(out=bigw[:], in_=wo[l, :, :, 0:512]), w=[big_d])
                                  for h in range(8):
                                      c, r0 = h // 2, (h % 2) * 64
                                      pn, pnd, pdn, pdnd = PB[4], PBd[4], PB[5], PBd[5]
                                      pb, pd = pp()
                                      T.op("dve", (lambda e, h=h: e.tensor_scalar(out=cm8[:], in0=ncumT[:], scalar1=identf[0:8, h:h + 1], scalar2=None, op0=ALU.mult)),
                                           r=[ncT_d], w=[cm8_d])
                                      T.op("pe", (lambda e, pb=pb: e.matmul(pb[:], lhsT=onesf[0:8, 0, :], rhs=cm8[:], start=True, stop=True)),
                                           r=[cm8_d], w=[pd])
                                      T.op("act", (lambda e, pb=pb: e.activation(out=cq[:], in_=pb[:], func=AF.Copy, scale=-1.0)), r=[pd], w=[cq_d])
                                      def foxS(j):
                                          ps, psd = scb()
                                          q0 = 128 * max(0, j - 4 * g)
                                          T.op("pe", (lambda e: e.matmul(ps[:, q0:512], lhsT=fk[r0:r0 + 64, c, j * 128:(j + 1) * 128],
                                                                        rhs=fq[r0:r0 + 64, c, q0:512], start=True, stop=True)),
                                               r=[fk_g[j // 4], fq_d], w=[psd])
                                          pi = j % 2
                                          T.op("dve", (lambda e: e.scalar_tensor_tensor(out=tmpf[pi][:, q0:512], in0=ps[:, q0:512], scalar=0.125, in1=cq[:, q0:512],
                                                                                       op0=ALU.mult, op1=ALU.add)),
                                               r=[psd, cq_d], w=[tmpf_d[pi]])
                                          if j >= 4 * g:
                                              jj = j - 4 * g
                                              T.op("dve", (lambda e: e.tensor_tensor(out=tmpf[pi][:, q0:512], in0=tmpf[pi][:, q0:512],
                                                                                    in1=nmask[:, DOFF[jj]:DOFF[jj] + 512 - q0], op=ALU.add)),
                                                   r=[tmpf_d[pi], dec_d], w=[tmpf_d[pi]])
                                          T.op("act", (lambda e: e.activation(out=pT[pi][:, q0:512], in_=tmpf[pi][:, q0:512], func=AF.Exp,
                                                                             bias=ncumK[:, j, h:h + 1], scale=1.0)),
                                               r=[tmpf_d[pi], ncK_g[j // 4]], w=[pT_d[pi]])

                                      def foxV(j):
                                          pi = j % 2
                                          q0 = 128 * max(0, j - 4 * g)
                                          T.op("pe", (lambda e: e.matmul(pn[r0:r0 + 64, q0:512], lhsT=fv[:, j, h * 64:(h + 1) * 64], rhs=pT[pi][:, q0:512],
                                                                        start=(j == 0), stop=(j == nkt - 1))),
                                               r=[fv_g[j // 4], pT_d[pi]], w=[pnd])
                                          T.op("pe", (lambda e: e.matmul(pdn[r0:r0 + 64, q0:512], lhsT=onesb[:, 0:64], rhs=pT[pi][:, q0:512],
                                                                        start=(j == 0), stop=(j == nkt - 1))),
                                               r=[pT_d[pi]], w=[pdnd])

                                      for j in range(nkt):
                                          foxS(j)
                                          if j > 0:
                                              foxV(j - 1)
                                      foxV(nkt - 1)
                                      if h % 2 == 1:
                                          T.op("dve", lambda e: e.reciprocal(out=rt1[:], in_=pdn[:]), r=[pdnd], w=[rt_d])
                                          T.op("dve", (lambda e, c=c: e.tensor_tensor(out=yy[:, 8 + c, :], in0=pn[:], in1=rt1[:], op=ALU.mult)),
                                               r=[pnd, rt_d], w=[yy_d[8 + c]])
                                  if g == 0 and b == 0:
                                      dbg(f"yy{l}", yy[:], yy_d)

                                  chk("E")
                                  for dc in range(8):
                                      wt, wd = mring.get(dc)
                                      for br in range(3):
                                          pa, pad = pp()
                                          for kc in range(4):
                                              T.op("pe", (lambda e, kc=kc, br=br, pa=pa, wt=wt: e.matmul(pa[:], lhsT=wt[0][:, br * 4 + kc, :], rhs=yy[:, br * 4 + kc, :],
                                                                                                    start=(kc == 0), stop=(kc == 3))),
                                                   r=[wd, yy_d[br * 4 + kc]], w=[pad])
                                          pg, pgd = pp()
                                          for kc in range(KC):
                                              T.op("pe", (lambda e, kc=kc, br=br, pg=pg, wt=wt: e.matmul(pg[:], lhsT=wt[0][:, 12 + br * 8 + kc, :], rhs=uT[:, kc, :],
                                                                                                    start=(kc == 0), stop=(kc == KC - 1))),
                                                   r=[wd, uT_d], w=[pgd])
                                          T.op("act", (lambda e, pg=pg, br=br, dc=dc: e.activation(out=sgt[:], in_=pg[:], func=AF.Sigmoid,
                                                                                                bias=bg_sb[:, br * 8 + dc:br * 8 + dc + 1], scale=1.0)),
                                               r=[pgd, ldep], w=[sgt_d])
                                          if br == 0:
                                              T.op("dve", (lambda e, pa=pa: e.tensor_tensor(out=macc[:], in0=pa[:], in1=sgt[:], op=ALU.mult)),
                                                   r=[pad, sgt_d], w=[macc_d])
                                          else:
                                              T.op("dve", (lambda e, pa=pa: e.tensor_tensor(out=sgt[:], in0=pa[:], in1=sgt[:], op=ALU.mult)),
                                                   r=[pad, sgt_d], w=[sgt_d])
                                              if br == 1:
                                                  T.op("dve", lambda e: e.tensor_tensor(out=macc[:], in0=macc[:], in1=sgt[:], op=ALU.add),
                                                       r=[macc_d, sgt_d], w=[macc_d])
                                              else:
                                                  T.op("dve", (lambda e, dc=dc: e.tensor_tensor(out=mrg[:, dc, :], in0=macc[:], in1=sgt[:], op=ALU.add)),
                                                       r=[macc_d, sgt_d], w=[mrg_d])

                                  chk("F")
                                  for tt in range(4):
                                      pb, pd = pp()
                                      for kc in range(KC):
                                          T.op("pe", (lambda e, kc=kc, pb=pb, tt=tt: e.matmul(
                                              pb[:], lhsT=mrg[:, kc, tt * 128:(tt + 1) * 128], rhs=bigw[:, kc, :],
                                              start=(kc == 0), stop=(kc == KC - 1))), r=[big_d, mrg_d], w=[pd])
                                      T.op("dve", (lambda e, pb=pb, tt=tt: e.tensor_tensor(out=acc[:, tt, :], in0=pb[:], in1=gb[:, 0:512], op=ALU.mult)),
                                           r=[pd, gbd], w=[acc_d])
                                  T.dma("pool", bigk, lambda e: e.dma_start(out=bigw[:], in_=wo[l, :, :, 512:1024]), w=[big_d])
                                  for tt in range(4):
                                      pb, pd = pp()
                                      for kc in range(KC):
                                          T.op("pe", (lambda e, kc=kc, pb=pb, tt=tt: e.matmul(
                                              pb[:], lhsT=mrg[:, kc, tt * 128:(tt + 1) * 128], rhs=bigw[:, kc, :],
                                              start=(kc == 0), stop=(kc == KC - 1))), r=[big_d, mrg_d], w=[pd])
                                      T.op("dve", (lambda e, pb=pb: e.tensor_tensor(out=rt1[:], in0=pb[:], in1=gb[:, 512:1024], op=ALU.mult)),
                                           r=[pd, gbd], w=[rt_d])
                                      xi = xload(xsrc[b, t0 + tt * 128:t0 + (tt + 1) * 128, :])
                                      T.op("dve", (lambda e, tt=tt, xi=xi: e.scalar_tensor_tensor(out=xg[xi][:, 0:512], in0=xg[xi][:, 0:512], scalar=DN_ALPHA,
                                                                                                in1=acc[:, tt, :], op0=ALU.mult, op1=ALU.add)),
                                           r=[acc_d, xg_d[xi]], w=[xg_d[xi]])
                                      T.op("dve", (lambda e, xi=xi: e.scalar_tensor_tensor(out=xg[xi][:, 512:1024], in0=xg[xi][:, 512:1024], scalar=DN_ALPHA,
                                                                                         in1=rt1[:], op0=ALU.mult, op1=ALU.add)),
                                           r=[rt_d, xg_d[xi]], w=[xg_d[xi]])
                                      ln_rows(xg[xi][:], xg_d[xi], stt[:, xi, :], st_d[xi])
                                      T.op("dve", (lambda e, xi=xi: e.tensor_scalar(out=xg[xi][:], in0=xg[xi][:], scalar1=stt[:, xi, 0:1],
                                                                                 scalar2=stt[:, xi, 2:3], op0=ALU.subtract, op1=ALU.mult)),
                                           r=[xg_d[xi], st_d[xi]], w=[xg_d[xi]])
                                      T.op("dve", (lambda e, xi=xi: e.tensor_tensor(out=xg[xi][:], in0=xg[xi][:], in1=lnA[:, 0, :], op=ALU.mult)),
                                           r=[xg_d[xi], lnA_d], w=[xg_d[xi]])
                                      T.op("dve", (lambda e, xi=xi: e.tensor_tensor(out=xg[xi][:], in0=xg[xi][:], in1=lnA[:, 1, :], op=ALU.add)),
                                           r=[xg_d[xi], lnA_d], w=[xg_d[xi]])
                                      if g == 0 and b == 0 and tt == 0:
                                          dbg(f"x1_{l}", xg[xi][:], [xg_d[xi]])
                                      T.dma("sp", f"xst{xi}", (lambda e, tt=tt, t0=t0, xi=xi: e.dma_start(out=x1[b, t0 + tt * 128:t0 + (tt + 1) * 128, :], in_=xg[xi][:])),
                                            r=[xg_d[xi]])
                              T.barrier()
                          T.barrier()

                          chk("G")
                          with ExitStack() as ms:
                              gb = sb(f"gbm{l}_{b}", [128, D], F32, ms)
                              gtmp = sb(f"gtmpm{l}_{b}", [128, 128], F32, ms)
                              lnM = sb(f"lnM{l}_{b}", [128, 2, D], F32, ms)
                              u2T = sb(f"u2T{l}_{b}", [128, KC, MU], BF16, ms)
                              u2fs = [sb(f"u2f{l}_{b}_{i}", [128, KC, 128], F32, ms) for i in range(2)]
                              xt = [sb(f"xt{l}_{b}_{i}", [128, D], F32, ms) for i in range(2)]
                              xns = [sb(f"xn{l}_{b}_{i}", [128, D], F32, ms) for i in range(2)]
                              stm = sb(f"stm{l}_{b}", [128, 2, 16], F32, ms)
                              yacc = sb(f"yacc{l}_{b}", [128, MT, D], F32, ms)
                              rS = sb(f"rS{l}_{b}", [128, MT, NE], F32, ms)
                              rSel = sb(f"rSel{l}_{b}", [128, MT, NE], F32, ms)
                              rP6 = sb(f"rP6{l}_{b}", [128, MT * 4, 6], F32, ms)
                              rG = sb(f"rG{l}_{b}", [128, MT, 4], F32, ms)
                              rGm = sb(f"rGm{l}_{b}", [128, MT], F32, ms)
                              rPen = sb(f"rPen{l}_{b}", [128, MT, 4], F32, ms)
                              rT8 = sb(f"rT8{l}_{b}", [128, MT, 8], F32, ms)
                              rW = sb(f"rW{l}_{b}", [128, MT, NE], F32, ms)
                              rSum = sb(f"rSum{l}_{b}", [128, MT], F32, ms)
                              wT = sb(f"wT{l}_{b}", [16, MU], F32, ms)
                              wTe = sb(f"wTe{l}_{b}", [16, 512], F32, ms)
                              wb_sb = [sb(f"wb{l}_{b}_{i}", [128, MU], BF16, ms) for i in range(2)]
                              silt = [sb(f"silt{l}_{b}_{i}", [128, 256], F32, ms) for i in range(2)]
                              hT = [sb(f"hT{l}_{b}_{i}", [128, 256], BF16, ms) for i in range(2)]
                              ering = Ring(T, ms, f"ex{l}_{b}_", [[128, KC, 1024]] * 3, BF16, 2, "pool", "ex")
                              (u2T_d, u2f_d0, xn_d0, yacc_d, rt_d, wT_d, wTe_d, gbd, gtd, lnM_d) = [Dep() for _ in range(10)]
                              u2f_ds = [Dep(), Dep()]
                              xn_ds = [Dep(), Dep()]
                              xt_d = [Dep(), Dep()]
                              stm_d = [Dep(), Dep()]
                              wb_d = [Dep(), Dep()]
                              silt_d = [Dep(), Dep()]
                              hT_d = [Dep(), Dep()]
                              for j in range(2):
                                  T.dma("sp", "lnps", (lambda e, j=j: e.dma_start(out=lnM[:, j, :], in_=lnp[l, 2 + j])), w=[lnM_d])
                              build_gb(gb, gtmp, 40, b, gbd, gtd)
                              for u in range(NU):
                                  u0 = u * MU
                                  srcs = []
                                  for e_ in range(NE):
                                      for hf in range(2):
                                          srcs.append(lambda t, e_=e_, hf=hf: [(t[0][:], we1[l, e_, hf]), (t[1][:], we3[l, e_, hf]), (t[2][:], we2[l, e_, hf])])
                                  ering.begin(srcs)
                                  ering.issue_upto(1)
                                  for ti in range(MT):
                                      xi = ti % 2
                                      xn, xn_d, u2f, u2f_d = xns[xi], xn_ds[xi], u2fs[xi], u2f_ds[xi]
                                      T.dma("sp", f"xld{xi}", (lambda e, xi=xi, ti=ti: e.dma_start(out=xt[xi][:], in_=x1[b, u0 + ti * 128:u0 + (ti + 1) * 128, :])),
                                            w=[xt_d[xi]])
                                      ln_rows(xt[xi][:], xt_d[xi], stm[:, xi, :], stm_d[xi])
                                      T.op("dve", (lambda e, xi=xi: e.tensor_scalar(out=xn[:], in0=xt[xi][:], scalar1=stm[:, xi, 0:1], scalar2=stm[:, xi, 2:3],
                                                                                 op0=ALU.subtract, op1=ALU.mult)), r=[xt_d[xi], stm_d[xi]], w=[xn_d])
                                      for kc in range(KC):
                                          pbk, pbd = PB[4 + kc // 4], PBd[4 + kc // 4]
                                          T.op("pe", (lambda e, kc=kc, pbk=pbk: e.transpose(out=pbk[:, (kc % 4) * 128:(kc % 4 + 1) * 128], in_=xn[:, kc * 128:(kc + 1) * 128],
                                                                                         identity=identf[:])), r=[xn_d], w=[pbd])
                                      for kc in range(KC):
                                          pbk, pbd = PB[4 + kc // 4], PBd[4 + kc // 4]
                                          T.op("act", (lambda e, kc=kc, pbk=pbk: e.activation(out=u2f[:, kc, :], in_=pbk[:, (kc % 4) * 128:(kc % 4 + 1) * 128], func=AF.Identity,
                                                                                           bias=adaT[:, 24 + kc, b:b + 1], scale=adaT[:, 32 + kc, b:b + 1])),
                                               r=[pbd, ldep], w=[u2f_d])
                                      T.op("dve", (lambda e, ti=ti: e.tensor_copy(out=u2T[:, :, ti * 128:(ti + 1) * 128], in_=u2f[:])), r=[u2f_d], w=[u2T_d])
                                      for kc in range(KC):
                                          T.op("pe", (lambda e, kc=kc, ti=ti: e.matmul(PB[6][:, ti * NE:(ti + 1) * NE], lhsT=u2f[:, kc, :], rhs=wr_sb[:, kc, :],
                                                                                    start=(kc == 0), stop=(kc == KC - 1))), r=[u2f_d], w=[PBd[6]])
                                  chk("MA")
                                  T.op("act", lambda e: e.activation(out=rS[:], in_=PB[6][:, 0:MT * NE].rearrange("p (a b) -> p a b", b=NE), func=AF.Sigmoid),
                                       r=[PBd[6]], w=[rt_d])
                                  T.op("dve", lambda e: e.tensor_tensor(out=rSel[:], in0=rS[:], in1=brb_sb[:, 0:MT, :], op=ALU.add), r=[rt_d], w=[rt_d])
                                  selv = rSel[:].rearrange("p a (g k) -> p (a g) k", k=4)
                                  for pi_, (a_, b_) in enumerate(((0, 1), (0, 2), (0, 3), (1, 2), (1, 3), (2, 3))):
                                      T.op("dve", (lambda e, pi_=pi_, a_=a_, b_=b_: e.tensor_tensor(out=rP6[:, :, pi_], in0=selv[:, :, a_], in1=selv[:, :, b_], op=ALU.add)),
                                           r=[rt_d], w=[rt_d])
                                  T.op("dve", lambda e: e.reduce_max(out=rG[:].rearrange("p a g -> p (a g)"), in_=rP6[:], axis=AX.X), r=[rt_d], w=[rt_d])
                                  T.op("dve", lambda e: e.reduce_max(out=rGm[:], in_=rG[:], axis=AX.X), r=[rt_d], w=[rt_d])
                                  T.op("dve", lambda e: e.tensor_tensor(out=rG[:], in0=rG[:], in1=rGm[:].unsqueeze(2).to_broadcast([128, MT, 4]), op=ALU.is_ge),
                                       r=[rt_d], w=[rt_d])
                                  T.op("dve", lambda e: e.tensor_scalar(out=rPen[:], in0=rG[:], scalar1=-1.0, scalar2=1.0e4, op0=ALU.add, op1=ALU.mult),
                                       r=[rt_d], w=[rt_d])
                                  selv4 = rSel[:].rearrange("p a (g k) -> p a g k", k=4)
                                  T.op("dve", lambda e: e.tensor_tensor(out=selv4, in0=selv4, in1=rG[:].unsqueeze(3).to_broadcast([128, MT, 4, 4]), op=ALU.mult),
                                       r=[rt_d], w=[rt_d])
                                  T.op("dve", lambda e: e.tensor_tensor(out=selv4, in0=selv4, in1=rPen[:].unsqueeze(3).to_broadcast([128, MT, 4, 4]), op=ALU.add),
                                       r=[rt_d], w=[rt_d])
                                  for ti in range(MT):
                                      T.op("dve", (lambda e, ti=ti: e.max(out=rT8[:, ti, :], in_=rSel[:, ti, :])), r=[rt_d], w=[rt_d])
                                  T.op("dve", lambda e: e.tensor_tensor(out=rW[:], in0=rSel[:], in1=rT8[:, :, 1:2].to_broadcast([128, MT, NE]), op=ALU.is_ge),
                                       r=[rt_d], w=[rt_d])
                                  T.op("dve", lambda e: e.tensor_tensor(out=rW[:], in0=rW[:], in1=rS[:], op=ALU.mult), r=[rt_d], w=[rt_d])
                                  T.op("dve", lambda e: e.reduce_sum(out=rSum[:], in_=rW[:], axis=AX.X), r=[rt_d], w=[rt_d])
                                  T.op("dve", lambda e: e.reciprocal(out=rSum[:], in_=rSum[:]), r=[rt_d], w=[rt_d])
                                  T.op("dve", lambda e: e.tensor_tensor(out=rW[:], in0=rW[:], in1=rSum[:].unsqueeze(2).to_broadcast([128, MT, NE]), op=ALU.mult),
                                       r=[rt_d], w=[rt_d])
                                  if u == 0 and b == 0:
                                      dbg(f"rW{l}", rW[:], [rt_d])
                                  chk("MB")
                                  for ti in range(MT):
                                      pbk, pbd = PB[4 + ti // 4], PBd[4 + ti // 4]
                                      T.op("pe", (lambda e, ti=ti, pbk=pbk: e.transpose(out=pbk[0:16, (ti % 4) * 128:(ti % 4 + 1) * 128], in_=rW[:, ti, :], identity=identf[:])),
                                           r=[rt_d], w=[pbd])
                                  for hh in range((MT + 3) // 4):
                                      n_ = min(4, MT - 4 * hh) * 128
                                      T.op("act", (lambda e, hh=hh, n_=n_: e.activation(out=wT[:, hh * 512:hh * 512 + n_], in_=PB[4 + hh][0:16, 0:n_], func=AF.Copy)),
                                           r=[PBd[4 + hh]], w=[wT_d])
                                  T.op("dve", lambda e: e.memset(yacc[:], 0.0), w=[yacc_d])
                                  chk("MC")
                                  steps = [(e_, hf, sg, m) for e_ in range(NE) for hf in range(2) for sg in range(NSG) for m in range(KC)]
                                  wts_ = {}

                                  def emitP(i):
                                      e_, hf, sg, m = steps[i]
                                      wbi = e_ % 2
                                      if hf == 0 and sg == 0 and m == 0:
                                          for hh in range(MU // 512):
                                              pbk, pbd = PB[6], PBd[6]
                                              T.op("dve", (lambda e, e_=e_, hh=hh: e.tensor_scalar(out=wTe[:], in0=wT[:, hh * 512:(hh + 1) * 512], scalar1=identf[0:16, e_:e_ + 1],
                                                                                                scalar2=None, op0=ALU.mult)), r=[wT_d], w=[wTe_d])
                                              T.op("pe", (lambda e: e.matmul(PB[6][:], lhsT=onesf[0:16, 0, :], rhs=wTe[:], start=True, stop=True)),
                                                   r=[wTe_d], w=[pbd])
                                              T.op("act", (lambda e, wbi=wbi, hh=hh: e.activation(out=wb_sb[wbi][:, hh * 512:(hh + 1) * 512], in_=PB[6][:], func=AF.Copy)),
                                                   r=[pbd], w=[wb_d[wbi]])
                                      if sg == 0 and m == 0:
                                          wts_[(e_, hf)] = ering.tile(e_ * 2 + hf)
                                      wt, wd = wts_[(e_, hf)]
                                      c0 = sg * 256
                                      si = i % 2
                                      pa, pad = PB[4 + si], PBd[4 + si]
                                      for kc in range(KC):
                                          T.op("pe", (lambda e, kc=kc: e.matmul(pa[:, 0:256], lhsT=wt[0][:, kc, m * 128:(m + 1) * 128],
                                                                               rhs=u2T[:, kc, c0:c0 + 256], start=(kc == 0), stop=(kc == KC - 1))),
                                               r=[wd, u2T_d], w=[pad])
                                      for kc in range(KC):
                                          T.op("pe", (lambda e, kc=kc: e.matmul(pa[:, 256:512], lhsT=wt[1][:, kc, m * 128:(m + 1) * 128],
                                                                               rhs=u2T[:, kc, c0:c0 + 256], start=(kc == 0), stop=(kc == KC - 1))),
                                               r=[wd, u2T_d], w=[pad])
                                      T.op("act", (lambda e: e.activation(out=silt[si][:], in_=pa[:, 0:256], func=AF.Silu)), r=[pad], w=[silt_d[si]])
                                      T.op("dve", (lambda e: e.tensor_tensor(out=silt[si][:], in0=silt[si][:], in1=wb_sb[wbi][:, c0:c0 + 256], op=ALU.mult)),
                                           r=[silt_d[si], wb_d[wbi]], w=[silt_d[si]])
                                      T.op("dve", (lambda e: e.tensor_tensor(out=hT[si][:], in0=pa[:, 256:512], in1=silt[si][:], op=ALU.mult)),
                                           r=[pad, silt_d[si]], w=[hT_d[si]])

                                  def emitY(i):
                                      e_, hf, sg, m = steps[i]
                                      wt, wd = wts_[(e_, hf)]
                                      si = i % 2
                                      for tt in range(2):
                                          for hv in range(2):
                                              yi = tt * 2 + hv
                                              T.op("pe", (lambda e, yi=yi, tt=tt, hv=hv: e.matmul(
                                                  PB[yi][:], lhsT=hT[si][:, tt * 128:(tt + 1) * 128], rhs=wt[2][:, m, hv * 512:(hv + 1) * 512],
                                                  start=(m == 0), stop=(m == KC - 1))), r=[wd, hT_d[si]], w=[PBd[yi]])
                                      if m == KC - 1:
                                          for tt in range(2):
                                              for hv in range(2):
                                                  yi = tt * 2 + hv
                                                  ti = sg * 2 + tt
                                                  T.op("dve", (lambda e, yi=yi, ti=ti, hv=hv: e.tensor_tensor(out=yacc[:, ti, hv * 512:(hv + 1) * 512],
                                                                                                            in0=PB[yi][:], in1=yacc[:, ti, hv * 512:(hv + 1) * 512], op=ALU.add)),
                                                       r=[PBd[yi], yacc_d], w=[yacc_d])

                                  emitP(0)
                                  for i in range(len(steps)):
                                      if i + 1 < len(steps):
                                          emitP(i + 1)
                                      emitY(i)
                                      if steps[i][2] == NSG - 1 and steps[i][3] == KC - 1:
                                          ering.issue_upto(steps[i][0] * 2 + steps[i][1] + 2)
                                  chk("MD")
                                  for ti in range(MT):
                                      xi = ti % 2
                                      T.dma("sp", f"xld{xi}", (lambda e, xi=xi, ti=ti: e.dma_start(out=xt[xi][:], in_=x1[b, u0 + ti * 128:u0 + (ti + 1) * 128, :])),
                                            w=[xt_d[xi]])
                                      T.op("dve", (lambda e, ti=ti: e.tensor_tensor(out=yacc[:, ti, :], in0=yacc[:, ti, :], in1=gb[:], op=ALU.mult)),
                                           r=[yacc_d, gbd], w=[yacc_d])
                                      T.op("dve", (lambda e, ti=ti, xi=xi: e.scalar_tensor_tensor(out=xt[xi][:], in0=xt[xi][:], scalar=DN_ALPHA, in1=yacc[:, ti, :],
                                                                                                op0=ALU.mult, op1=ALU.add)),
                                           r=[xt_d[xi], yacc_d], w=[xt_d[xi]])
                                      ln_rows(xt[xi][:], xt_d[xi], stm[:, xi, :], stm_d[xi])
                                      T.op("dve", (lambda e, xi=xi: e.tensor_scalar(out=xt[xi][:], in0=xt[xi][:], scalar1=stm[:, xi, 0:1], scalar2=stm[:, xi, 2:3],
                                                                                 op0=ALU.subtract, op1=ALU.mult)), r=[xt_d[xi], stm_d[xi]], w=[xt_d[xi]])
                                      T.op("dve", (lambda e, xi=xi: e.tensor_tensor(out=xt[xi][:], in0=xt[xi][:], in1=lnM[:, 0, :], op=ALU.mult)),
                                           r=[xt_d[xi], lnM_d], w=[xt_d[xi]])
                                      T.op("dve", (lambda e, xi=xi: e.tensor_tensor(out=xt[xi][:], in0=xt[xi][:], in1=lnM[:, 1, :], op=ALU.add)),
                                           r=[xt_d[xi], lnM_d], w=[xt_d[xi]])
                                      T.dma("sp", f"xst{xi}", (lambda e, xi=xi, ti=ti: e.dma_start(out=xdst[b, u0 + ti * 128:u0 + (ti + 1) * 128, :], in_=xt[xi][:])),
                                            r=[xt_d[xi]])
                              T.barrier()
                          T.barrier()
                  T.barrier()
              xsrc = xs[1]
        except _Stop:
            pass
        T.barrier()
    return nc, T.nins


def _consts(S):
    bf = ml_dtypes.bfloat16
    c = {}
    c["c_identb"] = np.eye(128, dtype=np.float32).astype(bf)
    c["c_identf"] = np.eye(128, dtype=np.float32)
    o = np.ones((128, 3, 128), np.float32)
    o[:, 1, :] = 1.0 / 512.0
    o[:, 2, :] = 1.0 / 128.0
    c["c_onesf"] = o
    P = np.zeros((128, 128), np.float32)
    for m in range(128):
        partner = m + 32 if (m % 64) < 32 else m - 32
        P[partner, m] = 1.0
    c["c_perm"] = P.astype(bf)
    half = 32
    inv = (10000.0 ** (-np.arange(half, dtype=np.float32) / half)).astype(np.float32)
    ang = np.arange(S, dtype=np.float32)[:, None] * inv[None, :]
    cos = np.cos(ang).T
    sin = np.sin(ang).T
    cs = np.zeros((128, 2, S), np.float32)
    for p in range(128):
        i = p % 32
        cs[p, 0] = cos[i]
        cs[p, 1] = -sin[i] if (p % 64) < 32 else sin[i]
    c["c_cs"] = cs.astype(bf)
    lg = np.log1p(-np.exp2(-5.0 - np.arange(4, dtype=np.float64)))
    DOFF = [0, 512, 896, 1152]
    dec = np.zeros((128, 7168), np.float64)
    ss = np.arange(128)[:, None]
    tt = np.arange(512)[None, :]
    for h in range(4):
        dec[:, h * 512:(h + 1) * 512] = np.exp(lg[h] * (tt - ss))
        for jj in range(4):
            s = 128 * jj + ss
            cs_, ct_ = s // 64, tt // 64
            same = cs_ == ct_
            earlier = cs_ < ct_
            v = np.where(same, np.exp(lg[h] * np.abs(tt - s)), np.where(earlier, np.exp(lg[h] * (tt - s).clip(min=0)), 0.0))
            o_ = 2048 + h * 1280 + DOFF[jj]
            dec[:, o_:o_ + 512 - 128 * jj] = v[:, 128 * jj:]
    c["c_dec"] = dec.astype(np.float32).astype(bf)
    nm = np.zeros((128, 1280), np.float32)
    for jj in range(4):
        nm[:, DOFF[jj]:DOFF[jj] + 512 - 128 * jj] = np.where(128 * jj + ss <= tt, 0.0, NEG)[:, 128 * jj:]
    c["c_nmask"] = nm.astype(bf)
    s8 = np.zeros((8, 8, 128), np.float32)
    for h in range(8):
        s8[h, h, :] = 1.0
    c["c_sel8"] = s8
    s16 = np.zeros((16, 16, 128), np.float32)
    for e in range(16):
        s16[e, e, :] = 1.0
    c["c_sel16"] = s16
    c["c_onesb"] = np.ones((128, 64), np.float32).astype(bf)
    c["c_gam"] = np.zeros((128, 64), np.float32)
    return c


def _kc_layout(w):
    sh = w.shape
    w = w.reshape(sh[:-2] + (KC, 128, sh[-1]))
    return np.ascontiguousarray(np.swapaxes(w, -3, -2))


def _prep_weights(inp, L):
    f = lambda a: np.asarray(a, dtype=np.float32)
    w = {}
    w_ada = f(inp["w_ada"])[:L]
    w["wada"] = np.ascontiguousarray(w_ada.reshape(L, KC, 128, 48, 128).transpose(0, 3, 2, 1, 4))
    w["bada"] = np.ascontiguousarray(f(inp["b_ada"])[:L].reshape(L, 48, 128).transpose(0, 2, 1))
    w_in = f(inp["w_in"])[:L]
    cols = []
    for c in range(4):
        cols.append(np.arange(A_GATE + c * 128, A_GATE + (c + 1) * 128))
        cols.append(np.arange(A_VAL + c * 128, A_VAL + (c + 1) * 128))
    for base, n in ((R_K, 2), (F_K, 4), (R_Q, 2), (F_Q, 4), (R_G, 4)):
        for c in range(n):
            cols.append(np.arange(base + c * 128, base + (c + 1) * 128))
    cols = np.stack(cols)
    wp = w_in[:, :, cols]
    w["wp"] = np.ascontiguousarray(wp.reshape(L, KC, 128, 24, 128).transpose(0, 3, 2, 1, 4))
    w["wf"] = _kc_layout(w_in[:, :, F_F:IN_COLS])
    w["wv"] = _kc_layout(np.concatenate([w_in[:, :, R_V:R_G], w_in[:, :, F_V:F_F]], axis=-1))
    wm = np.zeros((L, 8, 128, 36, 128), np.float32)
    for br, nm in enumerate(("w_conv_out", "w_ret_out", "w_fox_out")):
        wb_ = f(inp[nm])[:L].reshape(L, 4, 128, 8, 128)
        wm[:, :, :, br * 4:(br + 1) * 4, :] = wb_.transpose(0, 3, 2, 1, 4)
    wg = f(inp["w_gate"])[:L].reshape(L, KC, 128, 3, 8, 128)
    for br in range(3):
        wm[:, :, :, 12 + br * 8:12 + (br + 1) * 8, :] = wg[:, :, :, br].transpose(0, 3, 2, 1, 4)
    w["wm"] = wm
    w["bg"] = np.ascontiguousarray(f(inp["b_gate"])[:L].reshape(L, 24, 128).transpose(0, 2, 1))
    w["wo"] = _kc_layout(f(inp["w_out"])[:L])
    w["cw"] = np.ascontiguousarray(f(inp["conv_w"])[:L].reshape(L, CONV_K, 4, 128).transpose(0, 3, 2, 1))
    cpr = np.stack([f(inp["conv_b"])[:L], f(inp["conv_ln_g"])[:L], f(inp["conv_ln_b"])[:L]], axis=1)
    w["cprm"] = np.ascontiguousarray(cpr.reshape(L, 3, 4, 128).transpose(0, 3, 1, 2))
    w["bfg"] = np.ascontiguousarray(f(inp["b_forget"])[:L].reshape(L, 8, 1))
    lnp = np.stack([f(inp["ln1_g"])[:L], f(inp["ln1_b"])[:L], f(inp["ln2_g"])[:L], f(inp["ln2_b"])[:L]], axis=1)
    w["lnp"] = np.ascontiguousarray(np.broadcast_to(lnp[:, :, None, :], (L, 4, 128, D)))
    w["wr"] = _kc_layout(f(inp["w_router"]))
    w["brb"] = np.ascontiguousarray(np.broadcast_to(f(inp["b_router"])[None, None, :], (128, 8, NE)))
    w1 = f(inp["w1"])[:L].reshape(L, NE, KC, 128, 2, 1024)
    w["we1"] = np.ascontiguousarray(w1.transpose(0, 1, 4, 3, 2, 5))
    w3 = f(inp["w3"])[:L].reshape(L, NE, KC, 128, 2, 1024)
    w["we3"] = np.ascontiguousarray(w3.transpose(0, 1, 4, 3, 2, 5))
    w2 = f(inp["w2"])[:L].reshape(L, NE, 2, KC, 128, 1024)
    w["we2"] = np.ascontiguousarray(w2.transpose(0, 1, 2, 4, 3, 5))
    return w


def run(inputs, ncores, L, dump=None, trace=False, stop=None):
    x = np.asarray(inputs["x"], dtype=np.float32)
    c = np.asarray(inputs["c"], dtype=np.float32)
    B, S, _ = x.shape
    NSEQ = B // ncores
    nc, nins = build(NSEQ, S, L, dump, stop)
    shared = _prep_weights(inputs, L)
    shared.update(_consts(S))
    in_maps = []
    for i in range(ncores):
        m = dict(shared)
        m["x"] = np.ascontiguousarray(x[i * NSEQ:(i + 1) * NSEQ])
        ci = c[i * NSEQ:(i + 1) * NSEQ]
        m["ct"] = np.ascontiguousarray(ci.reshape(NSEQ, KC, 128).transpose(2, 1, 0))
        in_maps.append(m)
    res = run_bass_kernel_spmd(nc, in_maps, core_ids=list(range(ncores)), trace=trace)
    out = np.concatenate([r["out"] for r in res.results], axis=0)
    return out, res, nins


def kernel(**inputs):
    out, _, _ = run(inputs, NCORES, DEPTH)
    return out.astype(np.float32)
```
